# Optimizing a Trainium2 kernel written in Bass

```python
import math
import jax, jax.numpy as jnp
from jax import lax
import numpy as np

D_MODEL = 1024
BATCH = 4
SEQ = 4096
DEPTH = 2

POOL_WINDOWS = (2, 4, 8, 16)
N_POOL_GROUPS = len(POOL_WINDOWS)
POOL_GROUP_DIM = D_MODEL // 8
POOL_WIDTH = N_POOL_GROUPS * POOL_GROUP_DIM

SSD_WIDTH = D_MODEL
SSD_HEAD_DIM = 64
SSD_HEADS = SSD_WIDTH // SSD_HEAD_DIM
SSD_GROUPS = 2
HEADS_PER_GROUP = SSD_HEADS // SSD_GROUPS
SSD_STATE = 128
SSD_CONV = 4
SSD_CHUNK = 128
CONV_DIM = SSD_WIDTH + 2 * SSD_GROUPS * SSD_STATE

MIX_WIDTH = POOL_WIDTH + SSD_WIDTH
IN_PROJ_DIM = POOL_WIDTH + SSD_WIDTH + CONV_DIM + SSD_HEADS

PEER_HEADS = 8
PEER_TOPK = 16
N_KEYS = 128
N_EXPERTS = N_KEYS * N_KEYS
D_KEY = 256
PEER_BLOCK = 128

N_MOD = 6
EPS = 1e-6

kernel_name = "hybrid_pool_ssd_peer_adaln"


def rmsnorm(x, g):
    xf = x.astype(jnp.float32)
    y = xf * lax.rsqrt(jnp.mean(xf * xf, axis=-1, keepdims=True) + EPS)
    return (y * g.astype(jnp.float32)).astype(x.dtype)


def modulate(h, shift, scale):
    return h * (1 + scale[:, None, :]) + shift[:, None, :]


def pool_mixer(u, pool_w, pool_b, pool_scale):
    Bsz, S, _ = u.shape
    ug = u.astype(jnp.float32).reshape(Bsz, S, N_POOL_GROUPS, POOL_GROUP_DIM)
    cs = jnp.cumsum(ug, axis=1)
    t = jnp.arange(S)
    pooled = []
    for gi, w in enumerate(POOL_WINDOWS):
        csg = cs[:, :, gi]
        cs_pad = jnp.pad(csg, ((0, 0), (w, 0), (0, 0)))
        win_sum = cs_pad[:, w:] - cs_pad[:, :S]
        count = jnp.minimum(t + 1, w).astype(jnp.float32)[None, :, None]
        pooled.append(win_sum / count)
    pooled = jnp.stack(pooled, axis=2)
    mixed = pooled - ug
    y = jnp.einsum("bsgc,gcd->bsgd", mixed, pool_w.astype(jnp.float32)) + pool_b.astype(jnp.float32)
    y = y * pool_scale.astype(jnp.float32)
    return y.reshape(Bsz, S, POOL_WIDTH).astype(u.dtype)


def ssd_chunked(xdt, a, b, c):
    Bsz, S, G, R, P = xdt.shape
    N = b.shape[-1]
    L = SSD_CHUNK
    nc = S // L
    xdt = xdt.reshape(Bsz, nc, L, G, R, P)
    b = b.reshape(Bsz, nc, L, G, N)
    c = c.reshape(Bsz, nc, L, G, N)
    a = a.reshape(Bsz, nc, L, G, R).transpose(0, 3, 4, 1, 2)
    a_cs = jnp.cumsum(a, axis=-1)

    causal = jnp.tril(jnp.ones((L, L), dtype=bool))
    seg = a_cs[..., :, None] - a_cs[..., None, :]
    decay = jnp.exp(jnp.where(causal, seg, -jnp.inf))
    cb = jnp.einsum("bclgn,bcsgn->bcgls", c, b)
    y_diag = jnp.einsum("bcgls,bgrcls,bcsgrp->bclgrp", cb, decay, xdt)

    decay_to_end = jnp.exp(a_cs[..., -1:] - a_cs)
    states = jnp.einsum("bclgn,bgrcl,bclgrp->bcgrpn", b, decay_to_end, xdt)
    chunk_decay = jnp.exp(a_cs[..., -1])

    def step(h, inp):
        st, dec = inp
        return h * dec[..., None, None] + st, h

    h0 = jnp.zeros((Bsz, G, R, P, N), jnp.float32)
    _, h_prev = lax.scan(step, h0, (jnp.moveaxis(states, 1, 0), jnp.moveaxis(chunk_decay, -1, 0)))
    y_off = jnp.einsum("bclgn,cbgrpn,bgrcl->bclgrp", c, h_prev, jnp.exp(a_cs))
    return (y_diag + y_off).reshape(Bsz, S, G, R, P)


def ssd_mixer(z, xbc, dt_raw, conv_w, conv_b, dt_bias, a_log, d_skip, ssd_norm):
    Bsz, S, _ = xbc.shape
    dtype = xbc.dtype
    xf = xbc.astype(jnp.float32)
    conv = lax.conv_general_dilated(
        xf, conv_w.astype(jnp.float32).reshape(SSD_CONV, 1, CONV_DIM),
        window_strides=(1,), padding=[(SSD_CONV - 1, 0)],
        dimension_numbers=("NWC", "WIO", "NWC"), feature_group_count=CONV_DIM)
    conv = jax.nn.silu(conv + conv_b.astype(jnp.float32))
    xs = conv[..., :SSD_WIDTH].reshape(Bsz, S, SSD_GROUPS, HEADS_PER_GROUP, SSD_HEAD_DIM)
    bs = conv[..., SSD_WIDTH:SSD_WIDTH + SSD_GROUPS * SSD_STATE].reshape(Bsz, S, SSD_GROUPS, SSD_STATE)
    cs = conv[..., SSD_WIDTH + SSD_GROUPS * SSD_STATE:].reshape(Bsz, S, SSD_GROUPS, SSD_STATE)

    dt = jax.nn.softplus(dt_raw.astype(jnp.float32) + dt_bias.astype(jnp.float32))
    dt = dt.reshape(Bsz, S, SSD_GROUPS, HEADS_PER_GROUP)
    A = -jnp.exp(a_log.astype(jnp.float32)).reshape(SSD_GROUPS, HEADS_PER_GROUP)
    y = ssd_chunked(xs * dt[..., None], dt * A, bs, cs)
    y = y + xs * d_skip.astype(jnp.float32).reshape(SSD_GROUPS, HEADS_PER_GROUP)[..., None]
    y = y.reshape(Bsz, S, SSD_WIDTH) * jax.nn.silu(z.astype(jnp.float32))
    return rmsnorm(y, ssd_norm).astype(dtype)


def peer(h, w_query, sub_keys1, sub_keys2, expert_down, expert_up):
    Bsz, S, D = h.shape
    T = Bsz * S
    hf = h.reshape(T, D)
    q = (hf @ w_query).reshape(T, PEER_HEADS, 2, D_KEY // 2)
    s1 = jnp.einsum("thk,hnk->thn", q[:, :, 0], sub_keys1).astype(jnp.float32)
    s2 = jnp.einsum("thk,hnk->thn", q[:, :, 1], sub_keys2).astype(jnp.float32)
    v1, i1 = lax.top_k(s1, PEER_TOPK)
    v2, i2 = lax.top_k(s2, PEER_TOPK)
    cand = (v1[..., :, None] + v2[..., None, :]).reshape(T, PEER_HEADS, PEER_TOPK * PEER_TOPK)
    cand_idx = (i1[..., :, None] * N_KEYS + i2[..., None, :]).reshape(T, PEER_HEADS, PEER_TOPK * PEER_TOPK)
    top_s, pos = lax.top_k(cand, PEER_TOPK)
    idx = jnp.take_along_axis(cand_idx, pos, axis=-1)
    gates = jax.nn.softmax(top_s, axis=-1).astype(h.dtype)

    nb = T // PEER_BLOCK

    def block_fn(args):
        hb, ib, gb = args
        u = expert_down[ib]
        act = jax.nn.gelu(jnp.einsum("td,thkd->thk", hb, u), approximate=False)
        v = expert_up[ib]
        return jnp.einsum("thk,thkd->td", gb * act, v)

    out = lax.map(block_fn, (hf.reshape(nb, PEER_BLOCK, D),
                             idx.reshape(nb, PEER_BLOCK, PEER_HEADS, PEER_TOPK),
                             gates.reshape(nb, PEER_BLOCK, PEER_HEADS, PEER_TOPK)))
    return out.reshape(Bsz, S, D)


def setup_inputs(seed: int = 0) -> dict:
    key = jax.random.key(seed)
    ks = jax.random.split(key, 24)

    def nrm(k, shape, s):
        return jax.random.normal(k, shape, jnp.float32) * s

    x = nrm(ks[0], (BATCH, SEQ, D_MODEL), 1.0)
    c = nrm(ks[1], (BATCH, D_MODEL), 1.0)
    w_ada = nrm(ks[2], (DEPTH, D_MODEL, N_MOD * D_MODEL), 0.5 * D_MODEL ** -0.5)
    b_ada = nrm(ks[3], (DEPTH, N_MOD * D_MODEL), 0.02)
    norm_mix = 1.0 + nrm(ks[4], (DEPTH, D_MODEL), 0.02)
    norm_ffn = 1.0 + nrm(ks[5], (DEPTH, D_MODEL), 0.02)
    w_in = nrm(ks[6], (DEPTH, D_MODEL, IN_PROJ_DIM), D_MODEL ** -0.5)
    pool_w = nrm(ks[7], (DEPTH, N_POOL_GROUPS, POOL_GROUP_DIM, POOL_GROUP_DIM), POOL_GROUP_DIM ** -0.5)
    pool_b = nrm(ks[8], (DEPTH, N_POOL_GROUPS, POOL_GROUP_DIM), 0.02)
    pool_scale = 1.0 + nrm(ks[9], (DEPTH, N_POOL_GROUPS, POOL_GROUP_DIM), 0.1)
    conv_w = nrm(ks[10], (DEPTH, SSD_CONV, CONV_DIM), SSD_CONV ** -0.5)
    conv_b = nrm(ks[11], (DEPTH, CONV_DIM), 0.02)
    dt0 = jnp.exp(jax.random.uniform(ks[12], (DEPTH, SSD_HEADS), jnp.float32,
                                     minval=math.log(1e-3), maxval=math.log(1e-1)))
    dt_bias = dt0 + jnp.log(-jnp.expm1(-dt0))
    a_log = jnp.log(jax.random.uniform(ks[13], (DEPTH, SSD_HEADS), jnp.float32, minval=1.0, maxval=16.0))
    d_skip = 1.0 + nrm(ks[14], (DEPTH, SSD_HEADS), 0.1)
    ssd_norm = 1.0 + nrm(ks[15], (DEPTH, SSD_WIDTH), 0.02)
    w_out = nrm(ks[16], (DEPTH, MIX_WIDTH, D_MODEL), MIX_WIDTH ** -0.5)
    w_query = nrm(ks[17], (DEPTH, D_MODEL, PEER_HEADS * D_KEY), D_MODEL ** -0.5)
    sub_keys1 = nrm(ks[18], (DEPTH, PEER_HEADS, N_KEYS, D_KEY // 2), (D_KEY // 2) ** -0.5)
    sub_keys2 = nrm(ks[19], (DEPTH, PEER_HEADS, N_KEYS, D_KEY // 2), (D_KEY // 2) ** -0.5)
    expert_down = nrm(ks[20], (DEPTH, N_EXPERTS, D_MODEL), D_MODEL ** -0.5)
    expert_up = nrm(ks[21], (DEPTH, N_EXPERTS, D_MODEL), PEER_HEADS ** -0.5)
    norm_final = 1.0 + nrm(ks[22], (D_MODEL,), 0.02)
    return {"x": x, "c": c, "w_ada": w_ada, "b_ada": b_ada, "norm_mix": norm_mix, "norm_ffn": norm_ffn,
            "w_in": w_in, "pool_w": pool_w, "pool_b": pool_b, "pool_scale": pool_scale,
            "conv_w": conv_w, "conv_b": conv_b, "dt_bias": dt_bias, "a_log": a_log, "d_skip": d_skip,
            "ssd_norm": ssd_norm, "w_out": w_out, "w_query": w_query, "sub_keys1": sub_keys1,
            "sub_keys2": sub_keys2, "expert_down": expert_down, "expert_up": expert_up,
            "norm_final": norm_final}


def reference(x, c, w_ada, b_ada, norm_mix, norm_ffn, w_in, pool_w, pool_b, pool_scale,
              conv_w, conv_b, dt_bias, a_log, d_skip, ssd_norm, w_out, w_query, sub_keys1,
              sub_keys2, expert_down, expert_up, norm_final):
    Bsz = x.shape[0]
    c_act = jax.nn.silu(c)
    s_z = POOL_WIDTH
    s_xbc = POOL_WIDTH + SSD_WIDTH
    s_dt = POOL_WIDTH + SSD_WIDTH + CONV_DIM
    for l in range(DEPTH):
        mod = (c_act @ w_ada[l] + b_ada[l]).reshape(Bsz, N_MOD, D_MODEL)
        shift_m, scale_m, gate_m, shift_f, scale_f, gate_f = [mod[:, i] for i in range(N_MOD)]

        h = modulate(rmsnorm(x, norm_mix[l]), shift_m, scale_m)
        proj = h @ w_in[l]
        y_pool = pool_mixer(proj[..., :s_z], pool_w[l], pool_b[l], pool_scale[l])
        y_ssd = ssd_mixer(proj[..., s_z:s_xbc], proj[..., s_xbc:s_dt], proj[..., s_dt:],
                          conv_w[l], conv_b[l], dt_bias[l], a_log[l], d_skip[l], ssd_norm[l])
        y = jnp.concatenate([y_pool, y_ssd], axis=-1) @ w_out[l]
        x = x + gate_m[:, None, :] * y

        h = modulate(rmsnorm(x, norm_ffn[l]), shift_f, scale_f)
        y = peer(h, w_query[l], sub_keys1[l], sub_keys2[l], expert_down[l], expert_up[l])
        x = x + gate_f[:, None, :] * y
    return rmsnorm(x, norm_final)
```

```python
import contextlib
import numpy as np
import concourse.bass as bass
import concourse.mybir as mybir
from concourse.bass_utils import run_bass_kernel_spmd

F32 = mybir.dt.float32
BF16 = mybir.dt.bfloat16
U32 = mybir.dt.uint32
F32R = mybir.dt.float32r
AF = mybir.ActivationFunctionType
ALU = mybir.AluOpType

D = 1024
NIN = 3088
EPS = 1e-6
NEG = -1.0e30
SEM_CAP = 12000
SAME_ENGINE_WAIT = True


class Buf:
    __slots__ = ("name", "w", "r", "excl", "dsem", "dcnt")

    def __init__(self, name, excl=False):
        self.name = name
        self.w = None
        self.r = {}
        self.excl = excl
        self.dsem = None
        self.dcnt = 0


class T:
    def __init__(self, t, b):
        self.t = t
        self.b = b

    def __getitem__(self, k):
        return self.t[k]


class Prog:
    def __init__(self, nc, es):
        self.nc = nc
        self.es = es
        self.E = {"pe": nc.tensor, "act": nc.scalar, "dve": nc.vector, "pool": nc.gpsimd, "sp": nc.sync}
        self.sem = {}
        self.cnt = {}
        self.waited = {k: {} for k in self.E}
        self.latest = {}
        self.nsem = 0
        for k in self.E:
            self._newsem(k)

    def _mksem(self, name):
        self.nsem += 1
        return self.es.enter_context(self.nc.semaphore("%s_%d" % (name, self.nsem)))

    def _newsem(self, k):
        self.sem[k] = self._mksem("s" + k)
        self.cnt[k] = 0

    def sb(self, name, shape, dt, scope=None):
        self.nsem += 1
        name = "%s_t%d" % (name, self.nsem)
        t = (scope or self.es).enter_context(self.nc.sbuf_tensor(name, list(shape), dt))
        return T(t, Buf(name))

    def ps(self, name, shape, dt):
        t = self.es.enter_context(self.nc.psum_tensor(name, list(shape), dt))
        return T(t, Buf(name, excl=True))

    @staticmethod
    def _b(x):
        return x.b if isinstance(x, T) else x

    def _wait(self, eng, r, w):
        toks = []
        for x in r:
            b = self._b(x)
            if b.w is not None:
                toks.append(b.w)
            if b.excl:
                toks.extend(b.r.values())
        for x in w:
            b = self._b(x)
            if b.w is not None and not b.r:
                toks.append(b.w)
            toks.extend(b.r.values())
        e = self.E[eng]
        wd = self.waited[eng]
        for (s, v) in toks:
            if (not SAME_ENGINE_WAIT or eng == "pe") and s is self.sem.get(eng):
                continue
            if wd.get(id(s), 0) >= v:
                continue
            e.wait_ge(s, v)
            wd[id(s)] = v

    def _commit(self, tok, r, w):
        self.latest[id(tok[0])] = tok
        for x in r:
            b = self._b(x)
            if b.excl:
                b.w = tok
                b.r = {}
            else:
                b.r[id(tok[0])] = tok
        for x in w:
            b = self._b(x)
            b.w = tok
            b.r = {}

    def op(self, eng, fn, r=(), w=()):
        self._wait(eng, r, w)
        inst = fn(self.E[eng])
        if self.cnt[eng] >= SEM_CAP:
            self._newsem(eng)
        self.cnt[eng] += 1
        inst.then_inc(self.sem[eng], 1)
        self._commit((self.sem[eng], self.cnt[eng]), r, w)

    def dma(self, q, fn, r=(), w=()):
        self._wait(q, r, w)
        b = self._b(w[0])
        if b.dsem is None or b.dcnt >= SEM_CAP // 16:
            b.dsem = self._mksem("d")
            b.dcnt = 0
        inst = fn(self.E[q])
        b.dcnt += 1
        inst.then_inc(b.dsem, 16)
        self._commit((b.dsem, 16 * b.dcnt), r, w)

    def barrier(self, engs=None):
        toks = list(self.latest.values())
        for eng in (engs or self.E):
            wd = self.waited[eng]
            for (s, v) in toks:
                if wd.get(id(s), 0) >= v:
                    continue
                self.E[eng].wait_ge(s, v)
                wd[id(s)] = v


def bc(ap, shape):
    return ap.to_broadcast(list(shape))


class LayerW:
    pass


NAMES_L = ["w_ada", "b_ada", "nmix_b", "nffn_b", "ssdn_b", "w_in", "w_out", "w_q", "pool_w", "pool_sb",
           "conv_wb", "ssd_small", "keysT", "e_down", "e_up"]
SHAPES_L = {"w_ada": [1024, 6144], "b_ada": [1, 6144], "nmix_b": [128, 1024], "nffn_b": [128, 1024],
            "ssdn_b": [128, 1024], "w_in": [1024, NIN], "w_out": [1536, 1024], "w_q": [1024, 2048],
            "pool_w": [4, 128, 128], "pool_sb": [128, 8], "conv_wb": [128, 60], "ssd_small": [128, 48],
            "keysT": [128, 2048], "e_down": [16384, 1024], "e_up": [16384, 1024]}


def build_program(cfgs, dbg=None, stages="all"):
    layers = [c["l"] for c in cfgs]
    NOMAX = max(c["NO"] for c in cfgs)
    NOUT = cfgs[-1]["NO"]
    nc = bass.Bass("TRN2", target_bir_lowering=False)
    es = contextlib.ExitStack()
    dr = {}

    def din(name, shape, dt=F32):
        dr[name] = nc.dram_tensor(name, list(shape), dt, kind="ExternalInput").ap()
        return dr[name]

    srcs = set()
    for c in cfgs:
        srcs.add(c["pre"]); srcs.add(c["own"])
    x_pre = din("x_pre", [cfgs[0]["NP"] * 128, D]) if "x_pre" in srcs else None
    x_own = din("x_own", [cfgs[0]["NO"] * 128, D]) if "x_own" in srcs else None
    flag_d = din("flag", [128, 1])
    invc_d = din("invc", [128, 3 * 512])
    cT_d = din("cT", [128, 8])
    nfin_d = din("nfin_b", [128, 1024])
    LW = {}
    for l in layers:
        LW[l] = {n: din(n + "_" + l, SHAPES_L[n]) for n in NAMES_L}
    x_out = nc.dram_tensor("x_out", [NOUT * 128, D], F32, kind="ExternalOutput").ap()
    modb = nc.dram_tensor("modb", [128, 6144], F32, kind="Internal").ap()
    xs1 = nc.dram_tensor("xs1", [NOMAX * 128, D], F32, kind="Internal").ap()
    xs2 = nc.dram_tensor("xs2", [NOMAX * 128, D], F32, kind="Internal").ap()
    dbg_out = {}
    if dbg:
        for n, shp in dbg.items():
            dbg_out[n] = nc.dram_tensor("dbg_" + n, list(shp), F32, kind="ExternalOutput").ap()

    with es:
        P = Prog(nc, es)
        modb_b = Buf("modb")
        xs1_b = Buf("xs1")
        xs2_b = Buf("xs2")
        xout_b = Buf("xout")
        dbg_b = Buf("dbg")

        def dump(name, src_T, src_ap):
            if name in dbg_out:
                P.dma("sp", lambda e: e.dma_start(out=dbg_out[name], in_=src_ap), r=[src_T], w=[dbg_b])

        ident_bf = P.sb("ident_bf", [128, 128], BF16)
        ident_f = P.sb("ident_f", [128, 128], F32)
        tri = P.sb("tri", [128, 128], F32)
        su = P.sb("su", [128, 128], F32)
        ones_f = P.sb("ones_f", [128, 128], F32)
        flag = P.sb("flag_sb", [128, 1], F32)
        invc = P.sb("invc_sb", [128, 3, 4, 128], F32)
        small = P.sb("ssd_small_sb", [128, 48], F32)
        conv_wb = P.sb("conv_wb_sb", [128, 12, 5], F32)
        pool_sb = P.sb("pool_sb_sb", [128, 8], F32)
        ps = [P.ps("psb%d" % i, [128, 512], F32) if i not in (2, 3) else None for i in range(7)]
        psA = es.enter_context(nc.psum_tensor("psA2", [128, 1024], F32))
        ps[2] = T(psA[:, 0:512], Buf("psA_lo", excl=True))
        ps[3] = T(psA[:, 512:1024], Buf("psA_hi", excl=True))
        pshf = T(psA[:, :], Buf("pshf", excl=True))
        ps_bf = P.ps("psbf", [128, 1024], BF16)

        P.op("pool", lambda e: e.memset(ones_f[:], 1.0), w=[ones_f])
        P.op("pool", lambda e: e.affine_select(out=ident_f[:], in_=ones_f[:], pattern=[[-1, 128]],
                                               compare_op=ALU.is_equal, fill=0.0, base=0, channel_multiplier=1),
             r=[ones_f], w=[ident_f])
        P.op("pool", lambda e: e.tensor_copy(out=ident_bf[:], in_=ident_f[:]), r=[ident_f], w=[ident_bf])
        P.op("pool", lambda e: e.affine_select(out=tri[:], in_=ones_f[:], pattern=[[1, 128]],
                                               compare_op=ALU.is_ge, fill=0.0, base=0, channel_multiplier=-1),
             r=[ones_f], w=[tri])
        P.op("pool", lambda e: e.affine_select(out=su[:], in_=ones_f[:], pattern=[[-1, 128]],
                                               compare_op=ALU.is_gt, fill=0.0, base=0, channel_multiplier=1),
             r=[ones_f], w=[su])
        P.dma("sp", lambda e: e.dma_start(out=flag[:], in_=flag_d[:, :]), w=[flag])
        P.dma("sp", lambda e: e.dma_start(out=invc[:].rearrange("p a g t -> p (a g t)"), in_=invc_d[:, :]), w=[invc])

        TB = {}
        tb_list = []
        for l in layers:
            tt = nc.dram_tensor("tbc_%s" % l, [16384, 2 * D], BF16, kind="Internal").ap()
            tbd, tbu = Buf("tbcd_%s" % l), Buf("tbcu_%s" % l)
            TB[l] = (tt, [tbd, tbu])
            tb_list.append((LW[l]["e_down"], tt[:, 0:D], tbd))
            tb_list.append((LW[l]["e_up"], tt[:, D:2 * D], tbu))
        with contextlib.ExitStack() as sc:
            CR = 4
            cst = [P.sb("cvt_in%d" % i, [128, CR, D], F32, sc) for i in range(3)]
            cbf = [P.sb("cvt_out%d" % i, [128, CR, D], BF16, sc) for i in range(3)]
            nchunk = 16384 // (128 * CR)
            it = 0
            for c in range(nchunk):
                for (src, dstt, dstb) in tb_list:
                    a_in, a_out = cst[it % 3], cbf[it % 3]
                    sv = src.rearrange("(c p r) d -> c p r d", p=128, r=CR)
                    dv = dstt.rearrange("(c p r) d -> c p r d", p=128, r=CR)
                    q = ["sp", "act"][it % 2]
                    P.dma(q, lambda e, a_in=a_in, sv=sv, c=c: e.dma_start(out=a_in[:], in_=sv[c]), w=[a_in])
                    if it % 2 == 0:
                        P.op("dve", lambda e, a_in=a_in, a_out=a_out: e.tensor_copy(out=a_out[:], in_=a_in[:]), r=[a_in], w=[a_out])
                    else:
                        P.op("act", lambda e, a_in=a_in, a_out=a_out: e.copy(out=a_out[:], in_=a_in[:]), r=[a_in], w=[a_out])
                    P.dma(q, lambda e, a_out=a_out, dv=dv, c=c: e.dma_start(out=dv[c], in_=a_out[:]), r=[a_out], w=[dstb])
                    it += 1
            P.barrier()

        for li, l in enumerate(layers):
            W = LW[l]
            cfg = cfgs[li]
            NP, NO = cfg["NP"], cfg["NO"]
            final_norm = cfg["final_norm"]
            last = (cfg["dst"] == "x_out")
            src_pre = {"x_pre": x_pre, "xs2": xs2, None: None}[cfg["pre"]]
            dst_final = x_out if last else xs2
            with contextlib.ExitStack() as sc:
                cact = P.sb("cact", [128, 8], F32, sc)
                crep = P.sb("crep", [128, 8, 128], F32, sc)
                wst = [P.sb("wada_st%d" % i, [128, 8, 512], F32, sc) for i in range(2)]
                brow = [P.sb("brow%d" % i, [1, 512], F32, sc) for i in range(2)]
                mst = [P.sb("mst%d" % i, [128, 512], F32, sc) for i in range(2)]
                P.dma("sp", lambda e: e.dma_start(out=cact[:], in_=cT_d[:, :]), w=[cact])
                P.op("act", lambda e: e.activation(out=cact[:], in_=cact[:], func=AF.Silu), r=[cact], w=[cact])
                for k in range(8):
                    P.op("dve", lambda e, k=k: e.tensor_copy(out=crep[:, k, :], in_=bc(cact[:, k:k + 1], [128, 128])),
                         r=[cact], w=[crep])
                wv = W["w_ada"].rearrange("(k p) n -> p k n", p=128)
                for cb in range(12):
                    st = wst[cb % 2]
                    br = brow[cb % 2]
                    ms = mst[cb % 2]
                    pb = ps[cb % 2]
                    q = "sp" if cb % 2 == 0 else "act"
                    P.dma(q, lambda e, st=st, cb=cb: e.dma_start(out=st[:], in_=wv[:, :, cb * 512:(cb + 1) * 512]), w=[st])
                    P.dma(q, lambda e, br=br, cb=cb: e.dma_start(out=br[:], in_=W["b_ada"][0:1, cb * 512:(cb + 1) * 512]), w=[br])
                    for k in range(8):
                        P.op("pe", lambda e, k=k, st=st, pb=pb: e.matmul(pb[:], lhsT=crep[:, k, :], rhs=st[:, k, :],
                                                                          start=(k == 0), stop=False),
                             r=[crep, st], w=[pb])
                    P.op("pe", lambda e, br=br, pb=pb: e.matmul(pb[:], lhsT=ones_f[0:1, :], rhs=br[0:1, :],
                                                                 start=False, stop=True), r=[ones_f, br], w=[pb])
                    P.op("act", lambda e, ms=ms, pb=pb: e.copy(out=ms[:], in_=pb[:]), r=[pb], w=[ms])
                    P.dma("sp", lambda e, ms=ms, cb=cb: e.dma_start(out=modb[:, cb * 512:(cb + 1) * 512], in_=ms[:]),
                          r=[ms], w=[modb_b])
                P.dma("sp", lambda e: e.dma_start(out=small[:], in_=W["ssd_small"][:, :]), w=[small])
                P.op("act", lambda e: e.activation(out=small[:, 16:32], in_=small[:, 16:32], func=AF.Exp), r=[small], w=[small])
                P.op("dve", lambda e: e.tensor_scalar(out=small[:, 16:32], in0=small[:, 16:32], scalar1=-1.0, scalar2=None,
                                                      op0=ALU.mult), r=[small], w=[small])
                P.dma("sp", lambda e: e.dma_start(out=conv_wb[:].rearrange("p j k -> p (j k)"), in_=W["conv_wb"][:, :]), w=[conv_wb])
                P.dma("sp", lambda e: e.dma_start(out=pool_sb[:], in_=W["pool_sb"][:, :]), w=[pool_sb])
                P.op("dve", lambda e: e.tensor_tensor(out=pool_sb[:, 4:8], in0=pool_sb[:, 4:8], in1=pool_sb[:, 0:4], op=ALU.mult),
                     r=[pool_sb], w=[pool_sb])
                P.barrier()

            with contextlib.ExitStack() as sc:
                w_in = P.sb("w_in_bf", [128, 8, NIN], BF16, sc)
                w_out = P.sb("w_out_bf", [128, 12, 1024], BF16, sc)
                pool_w = P.sb("pool_w_bf", [128, 4, 128], BF16, sc)
                gm_b = P.sb("gm_b", [128, 1024], F32, sc)
                shm_b = P.sb("shm_b", [128, 1024], F32, sc)
                ssdn_b = P.sb("ssdn_b", [128, 1024], F32, sc)
                xt = [P.sb("xt%d" % i, [128, 1024], F32, sc) for i in range(2)]
                f32a = P.sb("f32a", [128, 1024], F32, sc)
                f32b = P.sb("f32b", [128, 1024], F32, sc)
                xs_tok = P.sb("xs_tok", [128, 16, 64], F32, sc)
                H = P.sb("H", [128, 16, 64], F32, sc)
                H_bf = P.sb("H_bf", [128, 1024], BF16, sc)
                junk_bf = P.sb("junk_bf", [128, 1024], BF16, sc)
                h_bf = P.sb("h_bf", [128, 1024], BF16, sc)
                hT = P.sb("hT", [128, 8, 128], BF16, sc)
                u_ext = P.sb("u_ext", [128, 4, 143], F32, sc)
                sA = P.sb("sA", [128, 4, 143], F32, sc)
                sB = P.sb("sB", [128, 4, 143], F32, sc)
                ptmp = P.sb("ptmp", [128, 4, 128], F32, sc)
                mixed = P.sb("mixed", [128, 4, 128], BF16, sc)
                ypT = P.sb("ypT", [128, 4, 128], BF16, sc)
                xbc = P.sb("xbc_ext", [128, 12, 131], F32, sc)
                cacc = P.sb("cacc", [128, 12, 128], F32, sc)
                bct = P.sb("bct", [128, 4, 128], BF16, sc)
                xdt = P.sb("xdt", [128, 16, 64], BF16, sc)
                xdte = P.sb("xdte", [128, 16, 64], BF16, sc)
                B_tok = P.sb("B_tok", [128, 256], BF16, sc)
                sm = P.sb("sm", [128, 8, 16], F32, sc)
                sm2 = P.sb("sm2", [128, 16], F32, sc)
                st1 = P.sb("st1", [128, 4], F32, sc)
                Rq = [P.sb("Rq%d" % i, [128, 4, 128], F32, sc) for i in range(2)]
                Dq = [P.sb("Dq%d" % i, [128, 4, 128], F32, sc) for i in range(2)]
                MTq = [P.sb("MTq%d" % i, [128, 4, 128], BF16, sc) for i in range(2)]
                CBm = P.sb("CBm", [128, 2, 128], F32, sc)
                yn_bf = P.sb("yn_bf", [128, 1024], BF16, sc)
                ynT = P.sb("ynT", [128, 8, 128], BF16, sc)

                P.dma("sp", lambda e: e.dma_start(out=gm_b[:], in_=modb[:, 1024:2048]), r=[modb_b], w=[gm_b])
                P.dma("sp", lambda e: e.dma_start(out=f32a[:], in_=W["nmix_b"][:, :]), w=[f32a])
                P.op("dve", lambda e: e.scalar_tensor_tensor(out=gm_b[:], in0=gm_b[:], scalar=1.0, in1=f32a[:],
                                                             op0=ALU.add, op1=ALU.mult), r=[gm_b, f32a], w=[gm_b])
                P.dma("sp", lambda e: e.dma_start(out=shm_b[:], in_=modb[:, 0:1024]), r=[modb_b], w=[shm_b])
                P.dma("sp", lambda e: e.dma_start(out=ssdn_b[:], in_=W["ssdn_b"][:, :]), w=[ssdn_b])
                gate_m = f32b
                P.dma("sp", lambda e: e.dma_start(out=gate_m[:], in_=modb[:, 2048:3072]), r=[modb_b], w=[gate_m])
                stg = [xt[0], xt[1], f32a, T(xs_tok.t, xs_tok.b)]
                stg_flat = [xt[0][:], xt[1][:], f32a[:], xs_tok[:].rearrange("p a b -> p (a b)")]
                pieces = []
                wiv = W["w_in"].rearrange("(k p) n -> p k n", p=128)
                for k in range(8):
                    for c0 in range(0, NIN, 1024):
                        c1 = min(NIN, c0 + 1024)
                        pieces.append((wiv[:, k, c0:c1], w_in, (k, c0, c1), None))
                wov = W["w_out"].rearrange("(k p) n -> p k n", p=128)
                for k in range(12):
                    pieces.append((wov[:, k, :], w_out, (k, 0, 1024), gate_m))
                cast_engs = ["dve", "pool", "act"]
                for i, (src, dstT, (k, c0, c1), mul) in enumerate(pieces):
                    sT = stg[i % 4]
                    sv = stg_flat[i % 4]
                    n = c1 - c0
                    q = ["sp", "act"][i % 2]
                    P.dma(q, lambda e, sv=sv, src=src, n=n: e.dma_start(out=sv[:, 0:n], in_=src), w=[sT])
                    if mul is None:
                        ce = cast_engs[i % 3]
                        if ce == "act":
                            P.op("act", lambda e, dstT=dstT, k=k, c0=c0, c1=c1, sv=sv, n=n:
                                 e.copy(out=dstT[:, k, c0:c1], in_=sv[:, 0:n]), r=[sT], w=[dstT])
                        else:
                            P.op(ce, lambda e, dstT=dstT, k=k, c0=c0, c1=c1, sv=sv, n=n:
                                 e.tensor_copy(out=dstT[:, k, c0:c1], in_=sv[:, 0:n]), r=[sT], w=[dstT])
                    else:
                        ce = ["dve", "pool"][i % 2]
                        P.op(ce, lambda e, dstT=dstT, k=k, c0=c0, c1=c1, sv=sv, n=n, mul=mul:
                             e.tensor_tensor(out=dstT[:, k, c0:c1], in0=sv[:, 0:n], in1=mul[:, 0:n], op=ALU.mult),
                             r=[sT, mul], w=[dstT])
                P.dma("sp", lambda e: e.dma_start(out=xt[0][:, 0:512].rearrange("p (g d) -> p g d", g=4),
                                                  in_=W["pool_w"].rearrange("g c d -> c g d")), w=[xt[0]])
                P.op("dve", lambda e: e.tensor_copy(out=pool_w[:].rearrange("p g d -> p (g d)"), in_=xt[0][:, 0:512]),
                     r=[xt[0]], w=[pool_w])
                P.op("pool", lambda e: e.memset(H[:], 0.0), w=[H])
                P.op("pool", lambda e: e.memset(H_bf[:], 0.0), w=[H_bf])
                P.op("pool", lambda e: e.memset(u_ext[:], 0.0), w=[u_ext])
                P.op("pool", lambda e: e.memset(xbc[:], 0.0), w=[xbc])

                def mix_tile(ti, src_ap, dst_ap, dst_b, full, invc_sel, apply_flag, need_u):
                    if isinstance(src_ap, tuple) and len(src_ap) == 2:
                        src_ap = [src_ap[0], src_ap[1]]
                    x_t = xt[ti % 2]
                    if apply_flag:
                        P.op("dve", lambda e: e.tensor_scalar(out=H[:], in0=H[:], scalar1=flag[:, 0:1], scalar2=None, op0=ALU.mult),
                             r=[H, flag], w=[H])
                        P.op("pool", lambda e: e.tensor_copy(out=H_bf[:], in_=H[:].rearrange("p a b -> p (a b)")), r=[H], w=[H_bf])
                        P.op("dve", lambda e: e.tensor_scalar(out=u_ext[:, :, 0:15], in0=u_ext[:, :, 0:15], scalar1=flag[:, 0:1],
                                                              scalar2=None, op0=ALU.mult), r=[u_ext, flag], w=[u_ext])
                        P.op("dve", lambda e: e.tensor_scalar(out=xbc[:, :, 0:3], in0=xbc[:, :, 0:3], scalar1=flag[:, 0:1],
                                                              scalar2=None, op0=ALU.mult), r=[xbc, flag], w=[xbc])
                    if isinstance(src_ap, tuple):
                        a_ap, b_ap, rb = src_ap
                        P.dma("sp", lambda e: e.dma_start(out=x_t[:], in_=a_ap), r=[rb], w=[x_t])
                        P.dma("act", lambda e: e.dma_start(out=f32b[:], in_=b_ap), r=[rb], w=[f32b])
                        P.op("dve", lambda e: e.copy_predicated(out=x_t[:], mask=maskT[:], data=f32b[:]), r=[maskT, f32b, x_t], w=[x_t])
                    else:
                        P.dma("sp", lambda e: e.dma_start(out=x_t[:], in_=src_ap[0]), r=[src_ap[1]] if src_ap[1] is not None else [], w=[x_t])
                    P.op("act", lambda e: e.activation(out=junk_bf[:], in_=x_t[:], func=AF.Square, accum_out=st1[:, 0:1]),
                         r=[x_t], w=[junk_bf, st1])
                    P.op("act", lambda e: e.activation(out=st1[:, 1:2], in_=st1[:, 0:1], func=AF.Sqrt, scale=1.0 / D, bias=EPS),
                         r=[st1], w=[st1])
                    P.op("dve", lambda e: e.reciprocal(out=st1[:, 1:2], in_=st1[:, 1:2]), r=[st1], w=[st1])
                    P.op("dve", lambda e: e.scalar_tensor_tensor(out=f32a[:], in0=x_t[:], scalar=st1[:, 1:2], in1=gm_b[:],
                                                                 op0=ALU.mult, op1=ALU.mult), r=[x_t, st1, gm_b], w=[f32a])
                    P.op("dve", lambda e: e.tensor_tensor(out=h_bf[:], in0=f32a[:], in1=shm_b[:], op=ALU.add),
                         r=[f32a, shm_b], w=[h_bf])
                    for k in range(8):
                        P.op("pe", lambda e, k=k: e.transpose(out=ps_bf[:, k * 128:(k + 1) * 128], in_=h_bf[:, k * 128:(k + 1) * 128],
                                                              identity=ident_bf[:]), r=[h_bf, ident_bf], w=[ps_bf])
                    P.op("act", lambda e: e.copy(out=hT[:].rearrange("p k t -> p (k t)"), in_=ps_bf[:]), r=[ps_bf], w=[hT])
                    groups = []
                    for gi in range(3):
                        groups.append(("x", [1536 + (gi * 4 + j) * 128 for j in range(4)], gi * 4))
                    if need_u:
                        groups.append(("u", [g * 128 for g in range(4)], 0))
                    for gidx, (kind, cols, j0) in enumerate(groups):
                        pb = ps[3 + gidx % 2]
                        for jj, c0 in enumerate(cols):
                            for k in range(8):
                                P.op("pe", lambda e, k=k, jj=jj, c0=c0, pb=pb: e.matmul(pb[:, jj * 128:(jj + 1) * 128],
                                                                                       lhsT=w_in[:, k, c0:c0 + 128], rhs=hT[:, k, :],
                                                                                       start=(k == 0), stop=(k == 7)),
                                     r=[hT, w_in], w=[pb])
                        if kind == "u":
                            P.op("act", lambda e, pb=pb: e.copy(out=u_ext[:, :, 15:143], in_=pb[:].rearrange("p (g t) -> p g t", g=4)),
                                 r=[pb], w=[u_ext])
                        else:
                            ce = "dve" if gidx % 2 == 0 else "act"
                            if ce == "act":
                                P.op("act", lambda e, pb=pb, j0=j0: e.copy(out=xbc[:, j0:j0 + 4, 3:131],
                                                                           in_=pb[:].rearrange("p (g t) -> p g t", g=4)), r=[pb], w=[xbc])
                            else:
                                P.op("dve", lambda e, pb=pb, j0=j0: e.tensor_copy(out=xbc[:, j0:j0 + 4, 3:131],
                                                                                  in_=pb[:].rearrange("p (g t) -> p g t", g=4)), r=[pb], w=[xbc])
                    if full:
                        for cb in range(2):
                            for k in range(8):
                                P.op("pe", lambda e, k=k, cb=cb: e.matmul(ps[1 + cb][:], lhsT=hT[:, k, :],
                                                                          rhs=w_in[:, k, 512 + cb * 512:1024 + cb * 512],
                                                                          start=(k == 0), stop=(k == 7)), r=[hT, w_in], w=[ps[1 + cb]])
                    for k in range(8):
                        P.op("pe", lambda e, k=k: e.matmul(ps[0][:, 0:16], lhsT=hT[:, k, :], rhs=w_in[:, k, 3072:3088],
                                                           start=(k == 0), stop=(k == 7)), r=[hT, w_in], w=[ps[0]])
                    if full:
                        for g in range(4):
                            w = 2 << g
                            cur, oth = u_ext, sA
                            step, lo = 1, 1
                            while step < w:
                                dstb = sA if cur is not sA else sB
                                P.op("pool", lambda e, g=g, cur=cur, dstb=dstb, lo=lo, step=step:
                                     e.tensor_tensor(out=dstb[:, g, lo:143], in0=cur[:, g, lo:143], in1=cur[:, g, lo - step:143 - step], op=ALU.add),
                                     r=[cur], w=[dstb])
                                cur = dstb
                                step *= 2
                                lo = 2 * step - 1
                            P.op("pool", lambda e, g=g, cur=cur: e.tensor_tensor(out=ptmp[:, g, :], in0=cur[:, g, 15:143],
                                                                                 in1=invc[:, invc_sel, g, :], op=ALU.mult),
                                 r=[cur, invc], w=[ptmp])
                            P.op("pool", lambda e, g=g: e.tensor_tensor(out=mixed[:, g, :], in0=ptmp[:, g, :], in1=u_ext[:, g, 15:143],
                                                                        op=ALU.subtract), r=[ptmp, u_ext], w=[mixed])
                    if need_u:
                        P.op("pool", lambda e: e.tensor_copy(out=u_ext[:, :, 0:15], in_=u_ext[:, :, 128:143]), r=[u_ext], w=[u_ext])
                    if full:
                        for g in range(4):
                            P.op("pe", lambda e, g=g: e.matmul(ps[5][:, g * 128:(g + 1) * 128], lhsT=pool_w[:, g, :], rhs=mixed[:, g, :],
                                                               start=True, stop=True), r=[pool_w, mixed], w=[ps[5]])
                        for g in range(4):
                            P.op("act", lambda e, g=g: e.activation(out=ypT[:, g, :], in_=ps[5][:, g * 128:(g + 1) * 128], func=AF.Identity,
                                                                    scale=pool_sb[:, g:g + 1], bias=pool_sb[:, 4 + g:5 + g]),
                                 r=[ps[5], pool_sb], w=[ypT])
                    nblk = 12 if full else 10
                    for j in range(nblk):
                        P.op("dve", lambda e, j=j: e.tensor_scalar(out=cacc[:, j, :], in0=xbc[:, j, 0:128], scalar1=conv_wb[:, j, 0:1],
                                                                   scalar2=conv_wb[:, j, 4:5], op0=ALU.mult, op1=ALU.add),
                             r=[xbc, conv_wb], w=[cacc])
                        for k in range(1, 4):
                            P.op("dve", lambda e, j=j, k=k: e.scalar_tensor_tensor(out=cacc[:, j, :], in0=xbc[:, j, k:k + 128],
                                                                                   scalar=conv_wb[:, j, k:k + 1], in1=cacc[:, j, :],
                                                                                   op0=ALU.mult, op1=ALU.add),
                                 r=[xbc, conv_wb, cacc], w=[cacc])
                    P.op("pool", lambda e: e.tensor_copy(out=xbc[:, :, 0:3], in_=xbc[:, :, 128:131]), r=[xbc], w=[xbc])
                    P.op("act", lambda e: e.activation(out=cacc[:, 0:nblk, :], in_=cacc[:, 0:nblk, :], func=AF.Silu), r=[cacc], w=[cacc])
                    if full:
                        P.op("pool", lambda e: e.tensor_copy(out=bct[:], in_=cacc[:, 8:12, :]), r=[cacc], w=[bct])
                    for half in range(2):
                        pb = ps[3 + half]
                        for jj in range(4):
                            j = half * 4 + jj
                            P.op("pe", lambda e, j=j, jj=jj, pb=pb: e.transpose(out=pb[:, jj * 128:(jj + 1) * 128], in_=cacc[:, j, :],
                                                                               identity=ident_f[:]), r=[cacc, ident_f], w=[pb])
                        P.op("act", lambda e, half=half, pb=pb: e.copy(out=xs_tok[:, half * 8:(half + 1) * 8, :].rearrange("p a b -> p (a b)"),
                                                                      in_=pb[:]), r=[pb], w=[xs_tok])
                    for g in range(2):
                        P.op("pe", lambda e, g=g: e.transpose(out=ps[5][:, g * 128:(g + 1) * 128], in_=cacc[:, 8 + g, :], identity=ident_f[:]),
                             r=[cacc, ident_f], w=[ps[5]])
                    P.op("act", lambda e: e.copy(out=B_tok[:], in_=ps[5][:, 0:256]), r=[ps[5]], w=[B_tok])
                    P.op("dve", lambda e: e.tensor_tensor(out=sm[:, 0, :], in0=ps[0][:, 0:16], in1=small[:, 0:16], op=ALU.add),
                         r=[ps[0], small], w=[sm])
                    P.op("act", lambda e: e.activation(out=sm[:, 0, :], in_=sm[:, 0, :], func=AF.Exp), r=[sm], w=[sm])
                    P.op("act", lambda e: e.activation(out=sm[:, 1, :], in_=sm[:, 0, :], func=AF.Ln, bias=1.0), r=[sm], w=[sm])
                    P.op("dve", lambda e: e.tensor_tensor(out=sm[:, 2, :], in0=sm[:, 1, :], in1=small[:, 16:32], op=ALU.mult),
                         r=[sm, small], w=[sm])
                    P.op("pe", lambda e: e.matmul(ps[0][:, 16:32], lhsT=tri[:], rhs=sm[:, 2, :], start=True, stop=True),
                         r=[tri, sm], w=[ps[0]])
                    P.op("pe", lambda e: e.matmul(ps[0][:, 32:48], lhsT=ones_f[:], rhs=sm[:, 2, :], start=True, stop=True),
                         r=[ones_f, sm], w=[ps[0]])
                    P.op("act", lambda e: e.copy(out=sm[:, 3:5, :].rearrange("p a b -> p (a b)"), in_=ps[0][:, 16:48]), r=[ps[0]], w=[sm])
                    P.op("act", lambda e: e.activation(out=sm[:, 5, :], in_=sm[:, 3, :], func=AF.Exp), r=[sm], w=[sm])
                    P.op("dve", lambda e: e.tensor_tensor(out=sm[:, 6, :], in0=sm[:, 4, :], in1=sm[:, 3, :], op=ALU.subtract), r=[sm], w=[sm])
                    P.op("act", lambda e: e.activation(out=sm[:, 6, :], in_=sm[:, 6, :], func=AF.Exp), r=[sm], w=[sm])
                    P.op("act", lambda e: e.activation(out=sm[:, 7, :], in_=sm[:, 4, :], func=AF.Exp), r=[sm], w=[sm])
                    P.op("dve", lambda e: e.tensor_tensor(out=sm2[:], in0=sm[:, 1, :], in1=sm[:, 6, :], op=ALU.mult), r=[sm], w=[sm2])
                    if full:
                        P.op("pool", lambda e: e.tensor_tensor(out=xdt[:], in0=xs_tok[:], in1=bc(sm[:, 1, :].unsqueeze(2), [128, 16, 64]),
                                                               op=ALU.mult), r=[xs_tok, sm], w=[xdt])
                    P.op("pool", lambda e: e.tensor_tensor(out=xdte[:], in0=xs_tok[:], in1=bc(sm2[:].unsqueeze(2), [128, 16, 64]),
                                                           op=ALU.mult), r=[xs_tok, sm2], w=[xdte])
                    if full:
                        for g in range(2):
                            P.op("pe", lambda e, g=g: e.matmul(ps[5][:, 256 + g * 128:384 + g * 128], lhsT=bct[:, g, :], rhs=bct[:, 2 + g, :],
                                                               start=True, stop=True), r=[bct], w=[ps[5]])
                        P.op("dve", lambda e: e.tensor_tensor(out=CBm[:], in0=ps[5][:, 256:512].rearrange("p (g t) -> p g t", g=2),
                                                              in1=bc(tri[:].unsqueeze(1), [128, 2, 128]), op=ALU.mult),
                             r=[ps[5], tri], w=[CBm])
                        t1 = f32a
                        for g in range(2):
                            pb = ps[5 + g]
                            P.op("pe", lambda e, g=g, pb=pb: e.matmul(pb[:], lhsT=bct[:, 2 + g, :], rhs=H_bf[:, g * 512:(g + 1) * 512],
                                                                      start=True, stop=True), r=[bct, H_bf], w=[pb])
                            P.op("dve", lambda e, g=g, pb=pb: e.tensor_tensor(out=t1[:, g * 512:(g + 1) * 512].rearrange("p (a b) -> p a b", a=8),
                                                                              in0=pb[:].rearrange("p (a b) -> p a b", a=8),
                                                                              in1=bc(sm[:, 5, g * 8:(g + 1) * 8].unsqueeze(2), [128, 8, 64]),
                                                                              op=ALU.mult), r=[pb, sm], w=[t1])
                        for q4 in range(4):
                            g = q4 // 2
                            Rb, Db, Mb = Rq[q4 % 2], Dq[q4 % 2], MTq[q4 % 2]
                            pseg = ps[5 + q4 % 2]
                            pyd = ps[3 + g]
                            P.op("dve", lambda e, q4=q4, Rb=Rb: e.tensor_tensor(out=Rb[:], in0=bc(tri[:].unsqueeze(1), [128, 4, 128]),
                                                                               in1=bc(sm[:, 2, 4 * q4:4 * q4 + 4].unsqueeze(2), [128, 4, 128]),
                                                                               op=ALU.mult), r=[tri, sm], w=[Rb])
                            P.op("pe", lambda e, Rb=Rb, pseg=pseg: e.matmul(pseg[:], lhsT=su[:], rhs=Rb[:].rearrange("p a b -> p (a b)"),
                                                                           start=True, stop=True), r=[su, Rb], w=[pseg])
                            P.op("act", lambda e, Db=Db, pseg=pseg: e.activation(out=Db[:].rearrange("p a b -> p (a b)"), in_=pseg[:], func=AF.Exp),
                                 r=[pseg], w=[Db])
                            P.op("pool", lambda e, Db=Db, Mb=Mb, g=g: e.tensor_tensor(out=Mb[:], in0=Db[:], in1=bc(CBm[:, g, :].unsqueeze(1), [128, 4, 128]),
                                                                                     op=ALU.mult), r=[Db, CBm], w=[Mb])
                            for hh in range(4):
                                h = 4 * q4 + hh
                                P.op("pe", lambda e, h=h, hh=hh, Mb=Mb, pyd=pyd: e.matmul(pyd[:, (h % 8) * 64:(h % 8 + 1) * 64], lhsT=Mb[:, hh, :],
                                                                                       rhs=xdt[:, h, :], start=True, stop=True), r=[Mb, xdt], w=[pyd])
                    for g in range(2):
                        pb = ps[5 + g]
                        P.op("pe", lambda e, g=g, pb=pb: e.matmul(pb[:], lhsT=B_tok[:, g * 128:(g + 1) * 128],
                                                                  rhs=xdte[:, g * 8:(g + 1) * 8, :].rearrange("p a b -> p (a b)"),
                                                                  start=True, stop=True), r=[B_tok, xdte], w=[pb])
                    P.op("dve", lambda e: e.tensor_tensor(out=H[:], in0=H[:], in1=bc(sm[:, 7, :].unsqueeze(2), [128, 16, 64]), op=ALU.mult),
                         r=[H, sm], w=[H])
                    for g in range(2):
                        pb = ps[5 + g]
                        P.op("dve", lambda e, g=g, pb=pb: e.tensor_tensor(out=H[:, g * 8:(g + 1) * 8, :], in0=H[:, g * 8:(g + 1) * 8, :],
                                                                          in1=pb[:].rearrange("p (a b) -> p a b", a=8), op=ALU.add),
                             r=[H, pb], w=[H])
                    P.op("pool", lambda e: e.tensor_copy(out=H_bf[:], in_=H[:].rearrange("p a b -> p (a b)")), r=[H], w=[H_bf])
                    if not full:
                        return
                    t1 = f32a
                    for g in range(2):
                        P.op("dve", lambda e, g=g: e.tensor_tensor(out=t1[:, g * 512:(g + 1) * 512], in0=t1[:, g * 512:(g + 1) * 512],
                                                                   in1=ps[3 + g][:], op=ALU.add), r=[t1, ps[3 + g]], w=[t1])
                    P.op("pool", lambda e: e.tensor_tensor(out=f32b[:].rearrange("p (a b) -> p a b", a=16), in0=xs_tok[:],
                                                           in1=bc(small[:, 32:48].unsqueeze(2), [128, 16, 64]), op=ALU.mult),
                         r=[xs_tok, small], w=[f32b])
                    P.op("pool", lambda e: e.tensor_tensor(out=t1[:], in0=t1[:], in1=f32b[:], op=ALU.add), r=[t1, f32b], w=[t1])
                    for g in range(2):
                        P.op("act", lambda e, g=g: e.activation(out=f32b[:, g * 512:(g + 1) * 512], in_=ps[1 + g][:], func=AF.Silu),
                             r=[ps[1 + g]], w=[f32b])
                    P.op("dve", lambda e: e.tensor_tensor(out=t1[:], in0=t1[:], in1=f32b[:], op=ALU.mult), r=[t1, f32b], w=[t1])
                    P.op("act", lambda e: e.activation(out=junk_bf[:], in_=t1[:], func=AF.Square, accum_out=st1[:, 2:3]),
                         r=[t1], w=[junk_bf, st1])
                    P.op("act", lambda e: e.activation(out=st1[:, 3:4], in_=st1[:, 2:3], func=AF.Sqrt, scale=1.0 / D, bias=EPS),
                         r=[st1], w=[st1])
                    P.op("dve", lambda e: e.reciprocal(out=st1[:, 3:4], in_=st1[:, 3:4]), r=[st1], w=[st1])
                    P.op("dve", lambda e: e.scalar_tensor_tensor(out=yn_bf[:], in0=t1[:], scalar=st1[:, 3:4], in1=ssdn_b[:],
                                                                 op0=ALU.mult, op1=ALU.mult), r=[t1, st1, ssdn_b], w=[yn_bf])
                    for k in range(8):
                        P.op("pe", lambda e, k=k: e.transpose(out=ps_bf[:, k * 128:(k + 1) * 128], in_=yn_bf[:, k * 128:(k + 1) * 128],
                                                              identity=ident_bf[:]), r=[yn_bf, ident_bf], w=[ps_bf])
                    P.op("act", lambda e: e.copy(out=ynT[:].rearrange("p k t -> p (k t)"), in_=ps_bf[:]), r=[ps_bf], w=[ynT])
                    for cb in range(2):
                        for kc in range(12):
                            lt = ypT[:, kc, :] if kc < 4 else ynT[:, kc - 4, :]
                            P.op("pe", lambda e, kc=kc, cb=cb, lt=lt: e.matmul(ps[1 + cb][:], lhsT=lt, rhs=w_out[:, kc, cb * 512:(cb + 1) * 512],
                                                                              start=(kc == 0), stop=(kc == 11)),
                                 r=[ypT, ynT, w_out], w=[ps[1 + cb]])
                        P.op("dve", lambda e, cb=cb: e.tensor_tensor(out=x_t[:, cb * 512:(cb + 1) * 512], in0=x_t[:, cb * 512:(cb + 1) * 512],
                                                                     in1=ps[1 + cb][:], op=ALU.add), r=[x_t, ps[1 + cb]], w=[x_t])
                    P.dma("sp", lambda e: e.dma_start(out=dst_ap, in_=x_t[:]), r=[x_t], w=[dst_b])

                maskT = None
                if cfg["own"] == "xs2sel":
                    maskT = P.sb("maskT", [128, 1024], mybir.dt.uint8, sc)
                    P.op("dve", lambda e: e.tensor_copy(out=maskT[:], in_=bc(flag[:, 0:1], [128, 1024])), r=[flag], w=[maskT])
                pre_b = xs2_b if cfg["pre"] == "xs2" else None
                for ti in range(NP):
                    mix_tile(ti, (src_pre[ti * 128:(ti + 1) * 128, :], pre_b), None, None, False, 1 if ti == 0 else 0, False, ti == NP - 1)

                def own_src(ti):
                    rr = slice(ti * 128, (ti + 1) * 128)
                    if cfg["own"] == "x_own":
                        return (x_own[rr, :], None)
                    if cfg["own"] == "xs2":
                        return (xs2[rr, :], xs2_b)
                    assert cfg["own"] == "xs2sel"
                    return (xs2[rr, :], xs2[NO * 128 + ti * 128:NO * 128 + (ti + 1) * 128, :], xs2_b)

                for ti in range(NO):
                    mix_tile(NP + ti, own_src(ti),
                             (x_out if stages == "mix" else xs1)[ti * 128:(ti + 1) * 128, :], xout_b if stages == "mix" else xs1_b, True,
                             (cfg["own0_sel"] if ti == 0 else 0), (ti == 0 and cfg["flag"]), True)
                P.barrier()

            if stages == "mix":
                continue
            with contextlib.ExitStack() as sc:
                NG = 8
                w_q = P.sb("w_q_bf", [128, 8, 2048], BF16, sc)
                keysT = P.sb("keysT_bf", [128, 16, 128], BF16, sc)
                gf_b = P.sb("gf_b", [128, 1024], F32, sc)
                shf_b = P.sb("shf_b", [128, 1024], F32, sc)
                gatef_b = P.sb("gatef_b", [128, 1024], F32, sc)
                nfin_b = P.sb("nfin_b", [128, 1024], F32, sc) if (last and final_norm) else None
                xp = [P.sb("xp%d" % i, [128, 1024], F32, sc) for i in range(2)]
                hf = [P.sb("hf%d" % i, [128, 1024], F32, sc) for i in range(2)]
                f32c = P.sb("f32c", [128, 1024], F32, sc)
                junk2 = P.sb("junk2", [128, 1024], BF16, sc)
                acc = P.sb("acc", [128, 1024], F32, sc)
                junk_bf = P.sb("junk_bf2", [128, 1024], BF16, sc)
                hf_bf = P.sb("hf_bf", [128, 1024], BF16, sc)
                hfT = P.sb("hfT", [128, 8, 128], BF16, sc)
                qT = P.sb("qT", [128, 16, 128], BF16, sc)
                scs = P.sb("scs", [128, 16, 128], F32, sc)
                wk = P.sb("wk", [128, 128], F32, sc)
                wk2 = P.sb("wk2", [128, 256], F32, sc)
                v = P.sb("v", [128, 8, 2, 16], F32, sc)
                iu = P.sb("iu", [128, 8, 2, 16], U32, sc)
                iff = P.sb("iff", [128, 8, 2, 16], F32, sc)
                cand = P.sb("cand", [128, 8, 16, 16], F32, sc)
                cidx = P.sb("cidx", [128, 8, 16, 16], F32, sc)
                tops = P.sb("tops", [128, 8, 16], F32, sc)
                gex = P.sb("gex", [128, 8, 16], F32, sc)
                gz = P.sb("gz", [128, 16], F32, sc)
                gates = [P.sb("gates%d" % i, [128, 128], F32, sc) for i in range(2)]
                idxf = P.sb("idxf", [128, 128], F32, sc)
                idxu = [P.sb("idxu%d" % i, [128, 128], U32, sc) for i in range(2)]
                avg = [P.sb("avg%d" % i, [128, 4], F32, sc) for i in range(2)]
                wgg = [P.sb("wgg%d" % i, [128, 4], F32, sc) for i in range(2)]
                st2 = P.sb("st2", [128, 4], F32, sc)
                ring = [P.sb("ring%d" % i, [128, 2048], BF16, sc) for i in range(NG)]
                diag = [P.sb("diag%d" % i, [128, 128], BF16, sc) for i in range(4)]
                rstate = {"n": 0}

                P.dma("sp", lambda e: e.dma_start(out=gf_b[:], in_=modb[:, 4096:5120]), r=[modb_b], w=[gf_b])
                P.dma("sp", lambda e: e.dma_start(out=f32c[:], in_=W["nffn_b"][:, :]), w=[f32c])
                P.op("dve", lambda e: e.scalar_tensor_tensor(out=gf_b[:], in0=gf_b[:], scalar=1.0, in1=f32c[:],
                                                             op0=ALU.add, op1=ALU.mult), r=[gf_b, f32c], w=[gf_b])
                P.dma("sp", lambda e: e.dma_start(out=shf_b[:], in_=modb[:, 3072:4096]), r=[modb_b], w=[shf_b])
                P.dma("sp", lambda e: e.dma_start(out=gatef_b[:], in_=modb[:, 5120:6144]), r=[modb_b], w=[gatef_b])
                if nfin_b is not None:
                    P.dma("sp", lambda e: e.dma_start(out=nfin_b[:], in_=nfin_d[:, :]), w=[nfin_b])
                stg = [xp[0], xp[1], hf[0], hf[1]]
                wqv = W["w_q"].rearrange("(k p) n -> p k n", p=128)
                i = 0
                for k in range(8):
                    for c0 in range(0, 2048, 1024):
                        sT = stg[i % 4]
                        q = ["sp", "act"][i % 2]
                        P.dma(q, lambda e, sT=sT, k=k, c0=c0: e.dma_start(out=sT[:], in_=wqv[:, k, c0:c0 + 1024]), w=[sT])
                        ce = ["dve", "pool"][i % 2]
                        P.op(ce, lambda e, sT=sT, k=k, c0=c0: e.tensor_copy(out=w_q[:, k, c0:c0 + 1024], in_=sT[:]), r=[sT], w=[w_q])
                        i += 1
                for c0 in range(0, 2048, 1024):
                    sT = stg[i % 4]
                    P.dma("sp", lambda e, sT=sT, c0=c0: e.dma_start(out=sT[:], in_=W["keysT"][:, c0:c0 + 1024]), w=[sT])
                    P.op("dve", lambda e, sT=sT, c0=c0: e.tensor_copy(out=keysT[:].rearrange("p b n -> p (b n)")[:, c0:c0 + 1024], in_=sT[:]),
                         r=[sT], w=[keysT])
                    i += 1

                def peer_A(ti):
                    b = ti % 2
                    x_t, hfx = xp[b], hf[b]
                    P.dma("sp", lambda e: e.dma_start(out=x_t[:], in_=xs1[ti * 128:(ti + 1) * 128, :]), r=[xs1_b], w=[x_t])
                    P.op("act", lambda e: e.activation(out=junk_bf[:], in_=x_t[:], func=AF.Square, accum_out=st2[:, 0:1]),
                         r=[x_t], w=[junk_bf, st2])
                    P.op("act", lambda e: e.activation(out=st2[:, 1:2], in_=st2[:, 0:1], func=AF.Sqrt, scale=1.0 / D, bias=EPS),
                         r=[st2], w=[st2])
                    P.op("dve", lambda e: e.reciprocal(out=st2[:, 1:2], in_=st2[:, 1:2]), r=[st2], w=[st2])
                    P.op("dve", lambda e: e.scalar_tensor_tensor(out=f32c[:], in0=x_t[:], scalar=st2[:, 1:2], in1=gf_b[:],
                                                                 op0=ALU.mult, op1=ALU.mult), r=[x_t, st2, gf_b], w=[f32c])
                    P.op("dve", lambda e: e.tensor_tensor(out=pshf[:], in0=f32c[:], in1=shf_b[:], op=ALU.add), r=[f32c, shf_b], w=[pshf])
                    P.op("act", lambda e: e.copy(out=hf_bf[:], in_=pshf[:]), r=[pshf], w=[hf_bf])
                    for k in range(8):
                        P.op("pe", lambda e, k=k: e.transpose(out=ps_bf[:, k * 128:(k + 1) * 128], in_=hf_bf[:, k * 128:(k + 1) * 128],
                                                              identity=ident_bf[:]), r=[hf_bf, ident_bf], w=[ps_bf])
                    P.op("act", lambda e: e.copy(out=hfT[:].rearrange("p k t -> p (k t)"), in_=ps_bf[:]), r=[ps_bf], w=[hfT])
                    for gq in range(4):
                        pb = ps[gq % 2]
                        for jj in range(4):
                            blk = gq * 4 + jj
                            for k in range(8):
                                P.op("pe", lambda e, k=k, jj=jj, blk=blk, pb=pb: e.matmul(pb[:, jj * 128:(jj + 1) * 128],
                                                                                          lhsT=w_q[:, k, blk * 128:(blk + 1) * 128], rhs=hfT[:, k, :],
                                                                                          start=(k == 0), stop=(k == 7)), r=[w_q, hfT], w=[pb])
                        P.op("act", lambda e, gq=gq, pb=pb: e.copy(out=qT[:, gq * 4:(gq + 1) * 4, :].rearrange("p a b -> p (a b)"), in_=pb[:]),
                             r=[pb], w=[qT])
                    for gq in range(4):
                        pb = ps[gq % 2]
                        for jj in range(4):
                            blk = gq * 4 + jj
                            P.op("pe", lambda e, jj=jj, blk=blk, pb=pb: e.matmul(pb[:, jj * 128:(jj + 1) * 128], lhsT=qT[:, blk, :],
                                                                                 rhs=keysT[:, blk, :], start=True, stop=True),
                                 r=[qT, keysT], w=[pb])
                        P.op("act", lambda e, gq=gq, pb=pb: e.copy(out=scs[:, gq * 4:(gq + 1) * 4, :].rearrange("p a b -> p (a b)"), in_=pb[:]),
                             r=[pb], w=[scs])
                    for blk in range(16):
                        h, s = blk // 2, blk % 2
                        P.op("dve", lambda e, blk=blk, h=h, s=s: e.max(out=v[:, h, s, 0:8], in_=scs[:, blk, :]), r=[scs], w=[v])
                        P.op("dve", lambda e, blk=blk, h=h, s=s: e.match_replace(out=wk[:], in_to_replace=v[:, h, s, 0:8],
                                                                                 in_values=scs[:, blk, :], imm_value=NEG), r=[scs, v], w=[wk])
                        P.op("dve", lambda e, h=h, s=s: e.max(out=v[:, h, s, 8:16], in_=wk[:]), r=[wk], w=[v])
                        P.op("dve", lambda e, blk=blk, h=h, s=s: e.max_index(out=iu[:, h, s, 0:8], in_max=v[:, h, s, 0:8],
                                                                             in_values=scs[:, blk, :]), r=[scs, v], w=[iu])
                        P.op("dve", lambda e, blk=blk, h=h, s=s: e.max_index(out=iu[:, h, s, 8:16], in_max=v[:, h, s, 8:16],
                                                                             in_values=scs[:, blk, :]), r=[scs, v], w=[iu])
                    P.op("dve", lambda e: e.tensor_copy(out=iff[:], in_=iu[:]), r=[iu], w=[iff])
                    P.op("dve", lambda e: e.tensor_scalar(out=iff[:, :, 0, :], in0=iff[:, :, 0, :], scalar1=128.0, scalar2=None, op0=ALU.mult),
                         r=[iff], w=[iff])
                    P.op("dve", lambda e: e.tensor_tensor(out=cand[:], in0=bc(v[:, :, 0, :].unsqueeze(3), [128, 8, 16, 16]),
                                                          in1=bc(v[:, :, 1, :].unsqueeze(2), [128, 8, 16, 16]), op=ALU.add), r=[v], w=[cand])
                    P.op("dve", lambda e: e.tensor_tensor(out=cidx[:], in0=bc(iff[:, :, 0, :].unsqueeze(3), [128, 8, 16, 16]),
                                                           in1=bc(iff[:, :, 1, :].unsqueeze(2), [128, 8, 16, 16]), op=ALU.add), r=[iff], w=[cidx])
                    for h in range(8):
                        ch = cand[:, h, :, :].rearrange("p a b -> p (a b)")
                        P.op("dve", lambda e, h=h, ch=ch: e.max(out=tops[:, h, 0:8], in_=ch), r=[cand], w=[tops])
                        P.op("dve", lambda e, h=h, ch=ch: e.match_replace(out=wk2[:], in_to_replace=tops[:, h, 0:8], in_values=ch, imm_value=NEG),
                             r=[cand, tops], w=[wk2])
                        P.op("dve", lambda e, h=h: e.max(out=tops[:, h, 8:16], in_=wk2[:]), r=[wk2], w=[tops])
                    for h in range(8):
                        ch = cand[:, h, :, :].rearrange("p a b -> p (a b)")
                        ci = cidx[:, h, :, :].rearrange("p a b -> p (a b)")
                        for k in range(16):
                            P.op("dve", lambda e, h=h, k=k, ch=ch, ci=ci: e.scalar_tensor_tensor(out=wk2[:], in0=ch, scalar=tops[:, h, k:k + 1], in1=ci,
                                                                                              op0=ALU.is_equal, op1=ALU.mult,
                                                                                              accum_out=idxf[:, h * 16 + k:h * 16 + k + 1]),
                                 r=[cand, cidx, tops], w=[wk2, idxf])
                    P.op("dve", lambda e: e.tensor_scalar(out=idxf[:], in0=idxf[:], scalar1=16383.0, scalar2=0.0, op0=ALU.min, op1=ALU.max),
                         r=[idxf], w=[idxf])
                    P.op("dve", lambda e: e.tensor_copy(out=idxu[b][:], in_=idxf[:]), r=[idxf], w=[idxu[b]])

                def peer_A2(ti):
                    b = ti % 2
                    P.op("dve", lambda e: e.tensor_tensor(out=gex[:], in0=tops[:], in1=bc(tops[:, :, 0:1], [128, 8, 16]), op=ALU.subtract),
                         r=[tops], w=[gex])
                    P.op("act", lambda e: e.activation(out=gex[:], in_=gex[:], func=AF.Exp), r=[gex], w=[gex])
                    P.op("dve", lambda e: e.tensor_reduce(out=gz[:, 0:8], in_=gex[:], axis=mybir.AxisListType.X, op=ALU.add), r=[gex], w=[gz])
                    P.op("dve", lambda e: e.reciprocal(out=gz[:, 8:16], in_=gz[:, 0:8]), r=[gz], w=[gz])
                    P.op("dve", lambda e: e.tensor_tensor(out=gates[b][:].rearrange("p (h k) -> p h k", h=8), in0=gex[:],
                                                          in1=bc(gz[:, 8:16].unsqueeze(2), [128, 8, 16]), op=ALU.mult), r=[gex, gz], w=[gates[b]])

                def gather(col, idxT):
                    tab, tabb = TB[l]
                    slot = ring[rstate["n"] % NG]
                    rstate["n"] += 1
                    P.dma("pool", lambda e: e.indirect_dma_start(out=slot[:], out_offset=None, in_=tab[:, :],
                                                                 in_offset=bass.IndirectOffsetOnAxis(ap=idxT[:, col:col + 1], axis=0)),
                          r=[idxT] + tabb, w=[slot])
                    return slot

                def peer_B(ti):
                    b = ti % 2
                    x_t = xp[b]
                    G = 4
                    LA = NG - G
                    slots = {}
                    issued = 0

                    def issue_upto(n):
                        nonlocal issued
                        while issued < min(n, 128):
                            slots[issued] = gather(issued, idxu[b])
                            issued += 1

                    for j in range(128):
                        issue_upto(j + LA)
                        sl = slots[j]
                        gp = (j // G) % 2
                        jj = j % G
                        P.op("dve", lambda e, sl=sl, gp=gp, jj=jj: e.scalar_tensor_tensor(out=junk2[:], in0=sl[:, 0:1024], scalar=1.0, in1=pshf[:],
                                                                                        op0=ALU.mult, op1=ALU.mult, accum_out=avg[gp][:, jj:jj + 1]),
                             r=[sl, pshf], w=[junk2, avg[gp]])
                        if jj == G - 1:
                            j0 = j - (G - 1)
                            P.op("act", lambda e, gp=gp: e.activation(out=wgg[gp][:], in_=avg[gp][:], func=AF.Gelu), r=[avg[gp]], w=[wgg[gp]])
                            P.op("dve", lambda e, gp=gp, j0=j0: e.tensor_tensor(out=wgg[gp][:], in0=wgg[gp][:], in1=gates[b][:, j0:j0 + G], op=ALU.mult),
                                 r=[wgg[gp], gates[b]], w=[wgg[gp]])
                            for k in range(G):
                                jk = j0 + k
                                slk = slots.pop(jk)
                                dg = diag[jk % 4]
                                P.op("act", lambda e, dg=dg, gp=gp, k=k: e.activation(out=dg[:], in_=ident_f[:], func=AF.Copy, scale=wgg[gp][:, k:k + 1]),
                                     r=[ident_f, wgg[gp]], w=[dg])
                                for hh in range(2):
                                    P.op("pe", lambda e, dg=dg, slk=slk, jk=jk, hh=hh: e.matmul(ps[4 + hh][:], lhsT=dg[:],
                                                                                            rhs=slk[:, 1024 + hh * 512:1024 + (hh + 1) * 512],
                                                                                            start=(jk == 0), stop=(jk == 127)),
                                         r=[dg, slk], w=[ps[4 + hh]])
                    for hh in range(2):
                        P.op("dve", lambda e, hh=hh: e.tensor_tensor(out=acc[:, hh * 512:(hh + 1) * 512], in0=ps[4 + hh][:],
                                                                     in1=gatef_b[:, hh * 512:(hh + 1) * 512], op=ALU.mult),
                             r=[ps[4 + hh], gatef_b], w=[acc])
                    P.op("dve", lambda e: e.tensor_tensor(out=x_t[:], in0=x_t[:], in1=acc[:], op=ALU.add), r=[x_t, acc], w=[x_t])
                    if last and final_norm:
                        P.op("act", lambda e: e.activation(out=junk_bf[:], in_=x_t[:], func=AF.Square, accum_out=st2[:, 2:3]),
                             r=[x_t], w=[junk_bf, st2])
                        P.op("act", lambda e: e.activation(out=st2[:, 3:4], in_=st2[:, 2:3], func=AF.Sqrt, scale=1.0 / D, bias=EPS),
                             r=[st2], w=[st2])
                        P.op("dve", lambda e: e.reciprocal(out=st2[:, 3:4], in_=st2[:, 3:4]), r=[st2], w=[st2])
                        P.op("dve", lambda e: e.scalar_tensor_tensor(out=x_t[:], in0=x_t[:], scalar=st2[:, 3:4], in1=nfin_b[:],
                                                                     op0=ALU.mult, op1=ALU.mult), r=[x_t, st2, nfin_b], w=[x_t])
                    dstb = xout_b if last else xs2_b
                    P.dma("sp", lambda e: e.dma_start(out=dst_final[ti * 128:(ti + 1) * 128, :], in_=x_t[:]), r=[x_t], w=[dstb])

                peer_A(0)
                peer_A2(0)
                for ti in range(NO):
                    peer_B(ti)
                    if ti + 1 < NO:
                        peer_A(ti + 1)
                        peer_A2(ti + 1)
                P.barrier()
        P.barrier(["sp"])
    return nc


def _layer_inputs(l, p):
    f = np.float32
    out = {}
    out["w_ada"] = np.ascontiguousarray(p["w_ada"][l], f)
    out["b_ada"] = np.ascontiguousarray(p["b_ada"][l][None, :], f)
    rep = lambda vec: np.ascontiguousarray(np.broadcast_to(np.asarray(vec, f)[None, :], (128, vec.shape[0])))
    out["nmix_b"] = rep(p["norm_mix"][l])
    out["nffn_b"] = rep(p["norm_ffn"][l])
    out["ssdn_b"] = rep(p["ssd_norm"][l])
    out["w_in"] = np.ascontiguousarray(p["w_in"][l], f)
    out["w_out"] = np.ascontiguousarray(p["w_out"][l], f)
    out["w_q"] = np.ascontiguousarray(p["w_query"][l], f)
    out["pool_w"] = np.ascontiguousarray(p["pool_w"][l], f)
    out["pool_sb"] = np.ascontiguousarray(np.concatenate([p["pool_scale"][l].T, p["pool_b"][l].T], axis=1), f)
    cw = p["conv_w"][l].T.reshape(12, 128, 4).transpose(1, 0, 2)
    cb = p["conv_b"][l].reshape(12, 128).T[:, :, None]
    out["conv_wb"] = np.ascontiguousarray(np.concatenate([cw, cb], axis=2).reshape(128, 60), f)
    out["ssd_small"] = rep(np.concatenate([p["dt_bias"][l], p["a_log"][l], p["d_skip"][l]]))
    k = np.stack([p["sub_keys1"][l], p["sub_keys2"][l]], axis=1)
    out["keysT"] = np.ascontiguousarray(k.transpose(3, 0, 1, 2).reshape(128, 2048), f)
    out["e_down"] = np.ascontiguousarray(p["expert_down"][l], f)
    out["e_up"] = np.ascontiguousarray(p["expert_up"][l], f)
    return out


def _invc_tables():
    t = np.arange(128)
    const = np.stack([np.full(128, 1.0 / w) for w in (2, 4, 8, 16)])
    start = np.stack([1.0 / np.minimum(t + 1, w) for w in (2, 4, 8, 16)])
    return const.astype(np.float32), start.astype(np.float32)


def _core_common(c_row, first_half):
    const, start = _invc_tables()
    own0 = start if first_half else const
    invc = np.concatenate([const.reshape(-1), start.reshape(-1), own0.reshape(-1)])
    return {
        "flag": np.full((128, 1), 0.0 if first_half else 1.0, np.float32),
        "invc": np.ascontiguousarray(np.broadcast_to(invc[None, :], (128, 1536)), np.float32),
        "cT": np.ascontiguousarray(c_row.reshape(8, 128).T, np.float32),
    }


_NC_CACHE = {}
FUSED = True


def _get_nc(key, cfgs):
    if key not in _NC_CACHE:
        _NC_CACHE[key] = build_program(cfgs)
    return _NC_CACHE[key]


def kernel(**p):
    p = {k: np.asarray(v) for k, v in p.items()}
    x = p["x"].astype(np.float32)
    Bsz, S, _ = x.shape
    HALF = S // 2
    NT = HALF // 128
    ncores = 2 * Bsz
    nfin = np.ascontiguousarray(np.broadcast_to(p["norm_final"].astype(np.float32)[None, :], (128, D)))
    if FUSED:
        cfgs = [dict(l="0", NP=0, NO=2 * NT, pre=None, own="x_own", flag=False, own0_sel=1, dst="xs2", final_norm=False),
                dict(l="1", NP=NT, NO=NT, pre="xs2", own="xs2sel", flag=True, own0_sel=2, dst="x_out", final_norm=True)]
        nc = _get_nc(("fused", NT), cfgs)
        lw = {}
        for l in range(2):
            lw.update({k + "_%d" % l: v for k, v in _layer_inputs(l, p).items()})
        in_maps = []
        for core in range(ncores):
            b, hh = core // 2, core % 2
            m = dict(lw)
            m.update(_core_common(p["c"][b].astype(np.float32), hh == 0))
            m["x_own"] = np.ascontiguousarray(x[b])
            m["nfin_b"] = nfin
            in_maps.append(m)
        res = run_bass_kernel_spmd(nc, in_maps, core_ids=list(range(ncores)))
        out = np.empty_like(x)
        for core in range(ncores):
            b, hh = core // 2, core % 2
            out[b, hh * HALF:(hh + 1) * HALF] = res.results[core]["x_out"]
        return out
    cur = x
    for l in range(2):
        last = (l == 1)
        cfgs = [dict(l="0", NP=NT, NO=NT, pre="x_pre", own="x_own", flag=True, own0_sel=2, dst="x_out", final_norm=last)]
        nc = _get_nc(("layer", last, NT), cfgs)
        lw = {k + "_0": v for k, v in _layer_inputs(l, p).items()}
        in_maps = []
        for core in range(ncores):
            b, hh = core // 2, core % 2
            m = dict(lw)
            m.update(_core_common(p["c"][b].astype(np.float32), hh == 0))
            m["x_own"] = np.ascontiguousarray(cur[b, hh * HALF:(hh + 1) * HALF])
            m["x_pre"] = np.ascontiguousarray(cur[b, 0:HALF])
            m["nfin_b"] = nfin
            in_maps.append(m)
        res = run_bass_kernel_spmd(nc, in_maps, core_ids=list(range(ncores)))
        nxt = np.empty_like(cur)
        for core in range(ncores):
            b, hh = core // 2, core % 2
            nxt[b, hh * HALF:(hh + 1) * HALF] = res.results[core]["x_out"]
        cur = nxt
    return cur
```

```python
import contextlib
import numpy as np
import concourse.bass as bass
import concourse.mybir as mybir
from concourse.bass_utils import run_bass_kernel_spmd

F32 = mybir.dt.float32
BF16 = mybir.dt.bfloat16
U32 = mybir.dt.uint32
F32R = mybir.dt.float32r
AF = mybir.ActivationFunctionType
ALU = mybir.AluOpType

D = 1024
NIN = 3088
EPS = 1e-6
NEG = -1.0e30
SEM_CAP = 12000
SAME_ENGINE_WAIT = True


class Buf:
    __slots__ = ("name", "w", "r", "excl", "dsem", "dcnt")

    def __init__(self, name, excl=False):
        self.name = name
        self.w = None
        self.r = {}
        self.excl = excl
        self.dsem = None
        self.dcnt = 0


class T:
    def __init__(self, t, b):
        self.t = t
        self.b = b

    def __getitem__(self, k):
        return self.t[k]


class Prog:
    def __init__(self, nc, es):
        self.nc = nc
        self.es = es
        self.E = {"pe": nc.tensor, "act": nc.scalar, "dve": nc.vector, "pool": nc.gpsimd, "sp": nc.sync}
        self.sem = {}
        self.cnt = {}
        self.waited = {k: {} for k in self.E}
        self.latest = {}
        self.nsem = 0
        for k in self.E:
            self._newsem(k)

    def _mksem(self, name):
        self.nsem += 1
        return self.es.enter_context(self.nc.semaphore("%s_%d" % (name, self.nsem)))

    def _newsem(self, k):
        self.sem[k] = self._mksem("s" + k)
        self.cnt[k] = 0

    def sb(self, name, shape, dt, scope=None):
        self.nsem += 1
        name = "%s_t%d" % (name, self.nsem)
        t = (scope or self.es).enter_context(self.nc.sbuf_tensor(name, list(shape), dt))
        return T(t, Buf(name))

    def ps(self, name, shape, dt):
        t = self.es.enter_context(self.nc.psum_tensor(name, list(shape), dt))
        return T(t, Buf(name, excl=True))

    @staticmethod
    def _b(x):
        return x.b if isinstance(x, T) else x

    def _wait(self, eng, r, w):
        toks = []
        for x in r:
            b = self._b(x)
            if b.w is not None:
                toks.append(b.w)
            if b.excl:
                toks.extend(b.r.values())
        for x in w:
            b = self._b(x)
            if b.w is not None and not b.r:
                toks.append(b.w)
            toks.extend(b.r.values())
        e = self.E[eng]
        wd = self.waited[eng]
        for (s, v) in toks:
            if (not SAME_ENGINE_WAIT or eng == "pe") and s is self.sem.get(eng):
                continue
            if wd.get(id(s), 0) >= v:
                continue
            e.wait_ge(s, v)
            wd[id(s)] = v

    def _commit(self, tok, r, w):
        self.latest[id(tok[0])] = tok
        for x in r:
            b = self._b(x)
            if b.excl:
                b.w = tok
                b.r = {}
            else:
                b.r[id(tok[0])] = tok
        for x in w:
            b = self._b(x)
            b.w = tok
            b.r = {}

    def op(self, eng, fn, r=(), w=()):
        self._wait(eng, r, w)
        inst = fn(self.E[eng])
        if self.cnt[eng] >= SEM_CAP:
            self._newsem(eng)
        self.cnt[eng] += 1
        inst.then_inc(self.sem[eng], 1)
        self._commit((self.sem[eng], self.cnt[eng]), r, w)

    def dma(self, q, fn, r=(), w=()):
        self._wait(q, r, w)
        b = self._b(w[0])
        if b.dsem is None or b.dcnt >= SEM_CAP // 16:
            b.dsem = self._mksem("d")
            b.dcnt = 0
        inst = fn(self.E[q])
        b.dcnt += 1
        inst.then_inc(b.dsem, 16)
        self._commit((b.dsem, 16 * b.dcnt), r, w)

    def barrier(self, engs=None):
        toks = list(self.latest.values())
        for eng in (engs or self.E):
            wd = self.waited[eng]
            for (s, v) in toks:
                if wd.get(id(s), 0) >= v:
                    continue
                self.E[eng].wait_ge(s, v)
                wd[id(s)] = v


def bc(ap, shape):
    return ap.to_broadcast(list(shape))


class LayerW:
    pass


NAMES_L = ["w_ada", "b_ada", "nmix_b", "nffn_b", "ssdn_b", "w_in", "w_out", "w_q", "pool_w", "pool_sb",
           "conv_wb", "ssd_small", "keysT", "e_down", "e_up"]
SHAPES_L = {"w_ada": [1024, 6144], "b_ada": [1, 6144], "nmix_b": [128, 1024], "nffn_b": [128, 1024],
            "ssdn_b": [128, 1024], "w_in": [1024, NIN], "w_out": [1536, 1024], "w_q": [1024, 2048],
            "pool_w": [4, 128, 128], "pool_sb": [128, 8], "conv_wb": [128, 60], "ssd_small": [128, 48],
            "keysT": [128, 2048], "e_down": [16384, 1024], "e_up": [16384, 1024]}


def build_program(cfgs, dbg=None, stages="all"):
    layers = [c["l"] for c in cfgs]
    NOMAX = max(c["NO"] for c in cfgs)
    NOUT = cfgs[-1]["NO"]
    nc = bass.Bass("TRN2", target_bir_lowering=False)
    es = contextlib.ExitStack()
    dr = {}

    def din(name, shape, dt=F32):
        dr[name] = nc.dram_tensor(name, list(shape), dt, kind="ExternalInput").ap()
        return dr[name]

    srcs = set()
    for c in cfgs:
        srcs.add(c["pre"]); srcs.add(c["own"])
    x_pre = din("x_pre", [cfgs[0]["NP"] * 128, D]) if "x_pre" in srcs else None
    x_own = din("x_own", [cfgs[0]["NO"] * 128, D]) if "x_own" in srcs else None
    flag_d = din("flag", [128, 1])
    invc_d = din("invc", [128, 3 * 512])
    cT_d = din("cT", [128, 8])
    nfin_d = din("nfin_b", [128, 1024])
    LW = {}
    for l in layers:
        LW[l] = {n: din(n + "_" + l, SHAPES_L[n]) for n in NAMES_L}
    x_out = nc.dram_tensor("x_out", [NOUT * 128, D], F32, kind="ExternalOutput").ap()
    modb = nc.dram_tensor("modb", [128, 6144], F32, kind="Internal").ap()
    xs1 = nc.dram_tensor("xs1", [NOMAX * 128, D], F32, kind="Internal").ap()
    xs2 = nc.dram_tensor("xs2", [NOMAX * 128, D], F32, kind="Internal").ap()
    dbg_out = {}
    if dbg:
        for n, shp in dbg.items():
            dbg_out[n] = nc.dram_tensor("dbg_" + n, list(shp), F32, kind="ExternalOutput").ap()

    with es:
        P = Prog(nc, es)
        modb_b = Buf("modb")
        xs1_b = Buf("xs1")
        xs2_b = Buf("xs2")
        xout_b = Buf("xout")
        dbg_b = Buf("dbg")

        def dump(name, src_T, src_ap):
            if name in dbg_out:
                P.dma("sp", lambda e: e.dma_start(out=dbg_out[name], in_=src_ap), r=[src_T], w=[dbg_b])

        ident_bf = P.sb("ident_bf", [128, 128], BF16)
        ident_f = P.sb("ident_f", [128, 128], F32)
        tri = P.sb("tri", [128, 128], F32)
        su = P.sb("su", [128, 128], F32)
        ones_f = P.sb("ones_f", [128, 128], F32)
        flag = P.sb("flag_sb", [128, 1], F32)
        invc = P.sb("invc_sb", [128, 3, 4, 128], F32)
        small = P.sb("ssd_small_sb", [128, 48], F32)
        conv_wb = P.sb("conv_wb_sb", [128, 12, 5], F32)
        pool_sb = P.sb("pool_sb_sb", [128, 8], F32)
        ps = [P.ps("psb%d" % i, [128, 512], F32) if i not in (2, 3) else None for i in range(7)]
        psA = es.enter_context(nc.psum_tensor("psA2", [128, 1024], F32))
        ps[2] = T(psA[:, 0:512], Buf("psA_lo", excl=True))
        ps[3] = T(psA[:, 512:1024], Buf("psA_hi", excl=True))
        pshf = T(psA[:, :], Buf("pshf", excl=True))
        ps_bf = P.ps("psbf", [128, 1024], BF16)

        P.op("pool", lambda e: e.memset(ones_f[:], 1.0), w=[ones_f])
        P.op("pool", lambda e: e.affine_select(out=ident_f[:], in_=ones_f[:], pattern=[[-1, 128]],
                                               compare_op=ALU.is_equal, fill=0.0, base=0, channel_multiplier=1),
             r=[ones_f], w=[ident_f])
        P.op("pool", lambda e: e.tensor_copy(out=ident_bf[:], in_=ident_f[:]), r=[ident_f], w=[ident_bf])
        P.op("pool", lambda e: e.affine_select(out=tri[:], in_=ones_f[:], pattern=[[1, 128]],
                                               compare_op=ALU.is_ge, fill=0.0, base=0, channel_multiplier=-1),
             r=[ones_f], w=[tri])
        P.op("pool", lambda e: e.affine_select(out=su[:], in_=ones_f[:], pattern=[[-1, 128]],
                                               compare_op=ALU.is_gt, fill=0.0, base=0, channel_multiplier=1),
             r=[ones_f], w=[su])
        P.dma("sp", lambda e: e.dma_start(out=flag[:], in_=flag_d[:, :]), w=[flag])
        P.dma("sp", lambda e: e.dma_start(out=invc[:].rearrange("p a g t -> p (a g t)"), in_=invc_d[:, :]), w=[invc])

        TB = {}
        tb_list = []
        for l in layers:
            for nm in ("e_down", "e_up"):
                tt = nc.dram_tensor("tb_%s_%s" % (nm, l), [16384, D], BF16, kind="Internal").ap()
                TB[(l, nm)] = (tt, Buf("tb_%s_%s" % (nm, l)))
                tb_list.append((LW[l][nm], tt, TB[(l, nm)][1]))
        def emit_conversion():
            with contextlib.ExitStack() as sc:
                CR = 4
                cst = [P.sb("cvt_in%d" % i, [128, CR, D], F32, sc) for i in range(3)]
                cbf = [P.sb("cvt_out%d" % i, [128, CR, D], BF16, sc) for i in range(3)]
                nchunk = 16384 // (128 * CR)
                it = 0
                for c in range(nchunk):
                    for (src, dstt, dstb) in tb_list:
                        a_in, a_out = cst[it % 3], cbf[it % 3]
                        sv = src.rearrange("(c p r) d -> c p r d", p=128, r=CR)
                        dv = dstt.rearrange("(c p r) d -> c p r d", p=128, r=CR)
                        q = ["sp", "act"][it % 2]
                        P.dma(q, lambda e, a_in=a_in, sv=sv, c=c: e.dma_start(out=a_in[:], in_=sv[c]), w=[a_in])
                        if it % 2 == 0:
                            P.op("dve", lambda e, a_in=a_in, a_out=a_out: e.tensor_copy(out=a_out[:], in_=a_in[:]), r=[a_in], w=[a_out])
                        else:
                            P.op("act", lambda e, a_in=a_in, a_out=a_out: e.copy(out=a_out[:], in_=a_in[:]), r=[a_in], w=[a_out])
                        P.dma(q, lambda e, a_out=a_out, dv=dv, c=c: e.dma_start(out=dv[c], in_=a_out[:]), r=[a_out], w=[dstb])
                        it += 1


        for li, l in enumerate(layers):
            W = LW[l]
            cfg = cfgs[li]
            NP, NO = cfg["NP"], cfg["NO"]
            final_norm = cfg["final_norm"]
            last = (cfg["dst"] == "x_out")
            src_pre = {"x_pre": x_pre, "xs2": xs2, None: None}[cfg["pre"]]
            dst_final = x_out if last else xs2
            with contextlib.ExitStack() as sc:
                cact = P.sb("cact", [128, 8], F32, sc)
                crep = P.sb("crep", [128, 8, 128], F32, sc)
                wst = [P.sb("wada_st%d" % i, [128, 8, 512], F32, sc) for i in range(2)]
                brow = [P.sb("brow%d" % i, [1, 512], F32, sc) for i in range(2)]
                mst = [P.sb("mst%d" % i, [128, 512], F32, sc) for i in range(2)]
                P.dma("sp", lambda e: e.dma_start(out=cact[:], in_=cT_d[:, :]), w=[cact])
                P.op("act", lambda e: e.activation(out=cact[:], in_=cact[:], func=AF.Silu), r=[cact], w=[cact])
                for k in range(8):
                    P.op("dve", lambda e, k=k: e.tensor_copy(out=crep[:, k, :], in_=bc(cact[:, k:k + 1], [128, 128])),
                         r=[cact], w=[crep])
                wv = W["w_ada"].rearrange("(k p) n -> p k n", p=128)
                for cb in range(12):
                    st = wst[cb % 2]
                    br = brow[cb % 2]
                    ms = mst[cb % 2]
                    pb = ps[cb % 2]
                    q = "sp" if cb % 2 == 0 else "act"
                    P.dma(q, lambda e, st=st, cb=cb: e.dma_start(out=st[:], in_=wv[:, :, cb * 512:(cb + 1) * 512]), w=[st])
                    P.dma(q, lambda e, br=br, cb=cb: e.dma_start(out=br[:], in_=W["b_ada"][0:1, cb * 512:(cb + 1) * 512]), w=[br])
                    for k in range(8):
                        P.op("pe", lambda e, k=k, st=st, pb=pb: e.matmul(pb[:], lhsT=crep[:, k, :], rhs=st[:, k, :],
                                                                          start=(k == 0), stop=False),
                             r=[crep, st], w=[pb])
                    P.op("pe", lambda e, br=br, pb=pb: e.matmul(pb[:], lhsT=ones_f[0:1, :], rhs=br[0:1, :],
                                                                 start=False, stop=True), r=[ones_f, br], w=[pb])
                    P.op("act", lambda e, ms=ms, pb=pb: e.copy(out=ms[:], in_=pb[:]), r=[pb], w=[ms])
                    P.dma("sp", lambda e, ms=ms, cb=cb: e.dma_start(out=modb[:, cb * 512:(cb + 1) * 512], in_=ms[:]),
                          r=[ms], w=[modb_b])
                P.dma("sp", lambda e: e.dma_start(out=small[:], in_=W["ssd_small"][:, :]), w=[small])
                P.op("act", lambda e: e.activation(out=small[:, 16:32], in_=small[:, 16:32], func=AF.Exp), r=[small], w=[small])
                P.op("dve", lambda e: e.tensor_scalar(out=small[:, 16:32], in0=small[:, 16:32], scalar1=-1.0, scalar2=None,
                                                      op0=ALU.mult), r=[small], w=[small])
                P.dma("sp", lambda e: e.dma_start(out=conv_wb[:].rearrange("p j k -> p (j k)"), in_=W["conv_wb"][:, :]), w=[conv_wb])
                P.dma("sp", lambda e: e.dma_start(out=pool_sb[:], in_=W["pool_sb"][:, :]), w=[pool_sb])
                P.op("dve", lambda e: e.tensor_tensor(out=pool_sb[:, 4:8], in0=pool_sb[:, 4:8], in1=pool_sb[:, 0:4], op=ALU.mult),
                     r=[pool_sb], w=[pool_sb])
                if li == 0:
                    emit_conversion()
                P.barrier()

            with contextlib.ExitStack() as sc:
                w_in = P.sb("w_in_bf", [128, 8, NIN], BF16, sc)
                w_out = P.sb("w_out_bf", [128, 12, 1024], BF16, sc)
                pool_w = P.sb("pool_w_bf", [128, 4, 128], BF16, sc)
                gm_b = P.sb("gm_b", [128, 1024], F32, sc)
                shm_b = P.sb("shm_b", [128, 1024], F32, sc)
                ssdn_b = P.sb("ssdn_b", [128, 1024], F32, sc)
                xt = [P.sb("xt%d" % i, [128, 1024], F32, sc) for i in range(2)]
                f32a = P.sb("f32a", [128, 1024], F32, sc)
                f32b = P.sb("f32b", [128, 1024], F32, sc)
                xs_tok = P.sb("xs_tok", [128, 16, 64], F32, sc)
                H = P.sb("H", [128, 16, 64], F32, sc)
                H_bf = P.sb("H_bf", [128, 1024], BF16, sc)
                junk_bf = P.sb("junk_bf", [128, 1024], BF16, sc)
                h_bf = P.sb("h_bf", [128, 1024], BF16, sc)
                hT = P.sb("hT", [128, 8, 128], BF16, sc)
                u_ext = P.sb("u_ext", [128, 4, 143], F32, sc)
                sA = P.sb("sA", [128, 4, 143], F32, sc)
                sB = P.sb("sB", [128, 4, 143], F32, sc)
                ptmp = P.sb("ptmp", [128, 4, 128], F32, sc)
                mixed = P.sb("mixed", [128, 4, 128], BF16, sc)
                ypT = P.sb("ypT", [128, 4, 128], BF16, sc)
                xbc = P.sb("xbc_ext", [128, 12, 131], F32, sc)
                cacc = P.sb("cacc", [128, 12, 128], F32, sc)
                bct = P.sb("bct", [128, 4, 128], BF16, sc)
                xdt = P.sb("xdt", [128, 16, 64], BF16, sc)
                xdte = P.sb("xdte", [128, 16, 64], BF16, sc)
                B_tok = P.sb("B_tok", [128, 256], BF16, sc)
                sm = P.sb("sm", [128, 8, 16], F32, sc)
                sm2 = P.sb("sm2", [128, 16], F32, sc)
                st1 = P.sb("st1", [128, 4], F32, sc)
                Rq = [P.sb("Rq%d" % i, [128, 4, 128], F32, sc) for i in range(2)]
                Dq = [P.sb("Dq%d" % i, [128, 4, 128], F32, sc) for i in range(2)]
                MTq = [P.sb("MTq%d" % i, [128, 4, 128], BF16, sc) for i in range(2)]
                CBm = P.sb("CBm", [128, 2, 128], F32, sc)
                yn_bf = P.sb("yn_bf", [128, 1024], BF16, sc)
                ynT = P.sb("ynT", [128, 8, 128], BF16, sc)

                P.dma("sp", lambda e: e.dma_start(out=gm_b[:], in_=modb[:, 1024:2048]), r=[modb_b], w=[gm_b])
                P.dma("sp", lambda e: e.dma_start(out=f32a[:], in_=W["nmix_b"][:, :]), w=[f32a])
                P.op("dve", lambda e: e.scalar_tensor_tensor(out=gm_b[:], in0=gm_b[:], scalar=1.0, in1=f32a[:],
                                                             op0=ALU.add, op1=ALU.mult), r=[gm_b, f32a], w=[gm_b])
                P.dma("sp", lambda e: e.dma_start(out=shm_b[:], in_=modb[:, 0:1024]), r=[modb_b], w=[shm_b])
                P.dma("sp", lambda e: e.dma_start(out=ssdn_b[:], in_=W["ssdn_b"][:, :]), w=[ssdn_b])
                gate_m = f32b
                P.dma("sp", lambda e: e.dma_start(out=gate_m[:], in_=modb[:, 2048:3072]), r=[modb_b], w=[gate_m])
                stg = [xt[0], xt[1], f32a, T(xs_tok.t, xs_tok.b)]
                stg_flat = [xt[0][:], xt[1][:], f32a[:], xs_tok[:].rearrange("p a b -> p (a b)")]
                pieces = []
                wiv = W["w_in"].rearrange("(k p) n -> p k n", p=128)
                for k in range(8):
                    for c0 in range(0, NIN, 1024):
                        c1 = min(NIN, c0 + 1024)
                        pieces.append((wiv[:, k, c0:c1], w_in, (k, c0, c1), None))
                wov = W["w_out"].rearrange("(k p) n -> p k n", p=128)
                for k in range(12):
                    pieces.append((wov[:, k, :], w_out, (k, 0, 1024), gate_m))
                cast_engs = ["dve", "pool", "act"]
                for i, (src, dstT, (k, c0, c1), mul) in enumerate(pieces):
                    sT = stg[i % 4]
                    sv = stg_flat[i % 4]
                    n = c1 - c0
                    q = ["sp", "act"][i % 2]
                    P.dma(q, lambda e, sv=sv, src=src, n=n: e.dma_start(out=sv[:, 0:n], in_=src), w=[sT])
                    if mul is None:
                        ce = cast_engs[i % 3]
                        if ce == "act":
                            P.op("act", lambda e, dstT=dstT, k=k, c0=c0, c1=c1, sv=sv, n=n:
                                 e.copy(out=dstT[:, k, c0:c1], in_=sv[:, 0:n]), r=[sT], w=[dstT])
                        else:
                            P.op(ce, lambda e, dstT=dstT, k=k, c0=c0, c1=c1, sv=sv, n=n:
                                 e.tensor_copy(out=dstT[:, k, c0:c1], in_=sv[:, 0:n]), r=[sT], w=[dstT])
                    else:
                        ce = ["dve", "pool"][i % 2]
                        P.op(ce, lambda e, dstT=dstT, k=k, c0=c0, c1=c1, sv=sv, n=n, mul=mul:
                             e.tensor_tensor(out=dstT[:, k, c0:c1], in0=sv[:, 0:n], in1=mul[:, 0:n], op=ALU.mult),
                             r=[sT, mul], w=[dstT])
                P.dma("sp", lambda e: e.dma_start(out=xt[0][:, 0:512].rearrange("p (g d) -> p g d", g=4),
                                                  in_=W["pool_w"].rearrange("g c d -> c g d")), w=[xt[0]])
                P.op("dve", lambda e: e.tensor_copy(out=pool_w[:].rearrange("p g d -> p (g d)"), in_=xt[0][:, 0:512]),
                     r=[xt[0]], w=[pool_w])
                P.op("pool", lambda e: e.memset(H[:], 0.0), w=[H])
                P.op("pool", lambda e: e.memset(H_bf[:], 0.0), w=[H_bf])
                P.op("pool", lambda e: e.memset(u_ext[:], 0.0), w=[u_ext])
                P.op("pool", lambda e: e.memset(xbc[:], 0.0), w=[xbc])

                def mix_tile(ti, src_ap, dst_ap, dst_b, full, invc_sel, apply_flag, need_u):
                    if isinstance(src_ap, tuple) and len(src_ap) == 2:
                        src_ap = [src_ap[0], src_ap[1]]
                    x_t = xt[ti % 2]
                    if apply_flag:
                        P.op("dve", lambda e: e.tensor_scalar(out=H[:], in0=H[:], scalar1=flag[:, 0:1], scalar2=None, op0=ALU.mult),
                             r=[H, flag], w=[H])
                        P.op("pool", lambda e: e.tensor_copy(out=H_bf[:], in_=H[:].rearrange("p a b -> p (a b)")), r=[H], w=[H_bf])
                        P.op("dve", lambda e: e.tensor_scalar(out=u_ext[:, :, 0:15], in0=u_ext[:, :, 0:15], scalar1=flag[:, 0:1],
                                                              scalar2=None, op0=ALU.mult), r=[u_ext, flag], w=[u_ext])
                        P.op("dve", lambda e: e.tensor_scalar(out=xbc[:, :, 0:3], in0=xbc[:, :, 0:3], scalar1=flag[:, 0:1],
                                                              scalar2=None, op0=ALU.mult), r=[xbc, flag], w=[xbc])
                    if isinstance(src_ap, tuple):
                        a_ap, b_ap, rb = src_ap
                        P.dma("sp", lambda e: e.dma_start(out=x_t[:], in_=a_ap), r=[rb], w=[x_t])
                        P.dma("act", lambda e: e.dma_start(out=f32b[:], in_=b_ap), r=[rb], w=[f32b])
                        P.op("dve", lambda e: e.copy_predicated(out=x_t[:], mask=maskT[:], data=f32b[:]), r=[maskT, f32b, x_t], w=[x_t])
                    else:
                        P.dma("sp", lambda e: e.dma_start(out=x_t[:], in_=src_ap[0]), r=[src_ap[1]] if src_ap[1] is not None else [], w=[x_t])
                    P.op("act", lambda e: e.activation(out=junk_bf[:], in_=x_t[:], func=AF.Square, accum_out=st1[:, 0:1]),
                         r=[x_t], w=[junk_bf, st1])
                    P.op("act", lambda e: e.activation(out=st1[:, 1:2], in_=st1[:, 0:1], func=AF.Sqrt, scale=1.0 / D, bias=EPS),
                         r=[st1], w=[st1])
                    P.op("dve", lambda e: e.reciprocal(out=st1[:, 1:2], in_=st1[:, 1:2]), r=[st1], w=[st1])
                    P.op("dve", lambda e: e.scalar_tensor_tensor(out=f32a[:], in0=x_t[:], scalar=st1[:, 1:2], in1=gm_b[:],
                                                                 op0=ALU.mult, op1=ALU.mult), r=[x_t, st1, gm_b], w=[f32a])
                    P.op("dve", lambda e: e.tensor_tensor(out=h_bf[:], in0=f32a[:], in1=shm_b[:], op=ALU.add),
                         r=[f32a, shm_b], w=[h_bf])
                    for k in range(8):
                        P.op("pe", lambda e, k=k: e.transpose(out=ps_bf[:, k * 128:(k + 1) * 128], in_=h_bf[:, k * 128:(k + 1) * 128],
                                                              identity=ident_bf[:]), r=[h_bf, ident_bf], w=[ps_bf])
                    P.op("act", lambda e: e.copy(out=hT[:].rearrange("p k t -> p (k t)"), in_=ps_bf[:]), r=[ps_bf], w=[hT])
                    groups = []
                    for gi in range(3):
                        groups.append(("x", [1536 + (gi * 4 + j) * 128 for j in range(4)], gi * 4))
                    if need_u:
                        groups.append(("u", [g * 128 for g in range(4)], 0))
                    for gidx, (kind, cols, j0) in enumerate(groups):
                        pb = ps[3 + gidx % 2]
                        for jj, c0 in enumerate(cols):
                            for k in range(8):
                                P.op("pe", lambda e, k=k, jj=jj, c0=c0, pb=pb: e.matmul(pb[:, jj * 128:(jj + 1) * 128],
                                                                                       lhsT=w_in[:, k, c0:c0 + 128], rhs=hT[:, k, :],
                                                                                       start=(k == 0), stop=(k == 7)),
                                     r=[hT, w_in], w=[pb])
                        if kind == "u":
                            P.op("act", lambda e, pb=pb: e.copy(out=u_ext[:, :, 15:143], in_=pb[:].rearrange("p (g t) -> p g t", g=4)),
                                 r=[pb], w=[u_ext])
                        else:
                            ce = "dve" if gidx % 2 == 0 else "act"
                            if ce == "act":
                                P.op("act", lambda e, pb=pb, j0=j0: e.copy(out=xbc[:, j0:j0 + 4, 3:131],
                                                                           in_=pb[:].rearrange("p (g t) -> p g t", g=4)), r=[pb], w=[xbc])
                            else:
                                P.op("dve", lambda e, pb=pb, j0=j0: e.tensor_copy(out=xbc[:, j0:j0 + 4, 3:131],
                                                                                  in_=pb[:].rearrange("p (g t) -> p g t", g=4)), r=[pb], w=[xbc])
                    if full:
                        for cb in range(2):
                            for k in range(8):
                                P.op("pe", lambda e, k=k, cb=cb: e.matmul(ps[1 + cb][:], lhsT=hT[:, k, :],
                                                                          rhs=w_in[:, k, 512 + cb * 512:1024 + cb * 512],
                                                                          start=(k == 0), stop=(k == 7)), r=[hT, w_in], w=[ps[1 + cb]])
                    for k in range(8):
                        P.op("pe", lambda e, k=k: e.matmul(ps[0][:, 0:16], lhsT=hT[:, k, :], rhs=w_in[:, k, 3072:3088],
                                                           start=(k == 0), stop=(k == 7)), r=[hT, w_in], w=[ps[0]])
                    if full:
                        for g in range(4):
                            w = 2 << g
                            cur, oth = u_ext, sA
                            step, lo = 1, 1
                            while step < w:
                                dstb = sA if cur is not sA else sB
                                P.op("pool", lambda e, g=g, cur=cur, dstb=dstb, lo=lo, step=step:
                                     e.tensor_tensor(out=dstb[:, g, lo:143], in0=cur[:, g, lo:143], in1=cur[:, g, lo - step:143 - step], op=ALU.add),
                                     r=[cur], w=[dstb])
                                cur = dstb
                                step *= 2
                                lo = 2 * step - 1
                            P.op("pool", lambda e, g=g, cur=cur: e.tensor_tensor(out=ptmp[:, g, :], in0=cur[:, g, 15:143],
                                                                                 in1=invc[:, invc_sel, g, :], op=ALU.mult),
                                 r=[cur, invc], w=[ptmp])
                            P.op("pool", lambda e, g=g: e.tensor_tensor(out=mixed[:, g, :], in0=ptmp[:, g, :], in1=u_ext[:, g, 15:143],
                                                                        op=ALU.subtract), r=[ptmp, u_ext], w=[mixed])
                    if need_u:
                        P.op("pool", lambda e: e.tensor_copy(out=u_ext[:, :, 0:15], in_=u_ext[:, :, 128:143]), r=[u_ext], w=[u_ext])
                    if full:
                        for g in range(4):
                            P.op("pe", lambda e, g=g: e.matmul(ps[5][:, g * 128:(g + 1) * 128], lhsT=pool_w[:, g, :], rhs=mixed[:, g, :],
                                                               start=True, stop=True), r=[pool_w, mixed], w=[ps[5]])
                        for g in range(4):
                            P.op("act", lambda e, g=g: e.activation(out=ypT[:, g, :], in_=ps[5][:, g * 128:(g + 1) * 128], func=AF.Identity,
                                                                    scale=pool_sb[:, g:g + 1], bias=pool_sb[:, 4 + g:5 + g]),
                                 r=[ps[5], pool_sb], w=[ypT])
                    nblk = 12 if full else 10
                    caccb = [Buf("cacc_blk%d" % j) for j in range(12)]
                    for j in range(nblk):
                        P.op("act", lambda e, j=j: e.activation(out=cacc[:, j, :], in_=xbc[:, j, 0:128], func=AF.Identity,
                                                                scale=conv_wb[:, j, 0:1], bias=conv_wb[:, j, 4:5]),
                             r=[xbc, conv_wb], w=([cacc, caccb[j]] if j == 0 else [caccb[j]]))
                    for j in range(nblk):
                        if j < 9:
                            for k in range(1, 4):
                                P.op("dve", lambda e, j=j, k=k: e.scalar_tensor_tensor(out=cacc[:, j, :], in0=xbc[:, j, k:k + 128],
                                                                                       scalar=conv_wb[:, j, k:k + 1], in1=cacc[:, j, :],
                                                                                       op0=ALU.mult, op1=ALU.add),
                                     r=[xbc, conv_wb, caccb[j]], w=[caccb[j]])
                        else:
                            for k in range(1, 4):
                                P.op("pool", lambda e, j=j, k=k: e.tensor_scalar(out=ptmp[:, 0, :], in0=xbc[:, j, k:k + 128],
                                                                                 scalar1=conv_wb[:, j, k:k + 1], scalar2=None, op0=ALU.mult),
                                     r=[xbc, conv_wb], w=[ptmp])
                                P.op("pool", lambda e, j=j: e.tensor_tensor(out=cacc[:, j, :], in0=cacc[:, j, :], in1=ptmp[:, 0, :], op=ALU.add),
                                     r=[ptmp, caccb[j]], w=[caccb[j]])
                    P.op("dve", lambda e: e.tensor_copy(out=st1[:, 0:1], in_=st1[:, 0:1]), r=caccb[:nblk] + [st1], w=[cacc, st1])
                    P.op("pool", lambda e: e.tensor_copy(out=xbc[:, :, 0:3], in_=xbc[:, :, 128:131]), r=[xbc], w=[xbc])
                    P.op("act", lambda e: e.activation(out=cacc[:, 0:nblk, :], in_=cacc[:, 0:nblk, :], func=AF.Silu), r=[cacc], w=[cacc])
                    if full:
                        P.op("pool", lambda e: e.tensor_copy(out=bct[:], in_=cacc[:, 8:12, :]), r=[cacc], w=[bct])
                    for half in range(2):
                        pb = ps[3 + half]
                        for jj in range(4):
                            j = half * 4 + jj
                            P.op("pe", lambda e, j=j, jj=jj, pb=pb: e.transpose(out=pb[:, jj * 128:(jj + 1) * 128], in_=cacc[:, j, :],
                                                                               identity=ident_f[:]), r=[cacc, ident_f], w=[pb])
                        P.op("act", lambda e, half=half, pb=pb: e.copy(out=xs_tok[:, half * 8:(half + 1) * 8, :].rearrange("p a b -> p (a b)"),
                                                                      in_=pb[:]), r=[pb], w=[xs_tok])
                    for g in range(2):
                        P.op("pe", lambda e, g=g: e.transpose(out=ps[5][:, g * 128:(g + 1) * 128], in_=cacc[:, 8 + g, :], identity=ident_f[:]),
                             r=[cacc, ident_f], w=[ps[5]])
                    P.op("act", lambda e: e.copy(out=B_tok[:], in_=ps[5][:, 0:256]), r=[ps[5]], w=[B_tok])
                    P.op("dve", lambda e: e.tensor_tensor(out=sm[:, 0, :], in0=ps[0][:, 0:16], in1=small[:, 0:16], op=ALU.add),
                         r=[ps[0], small], w=[sm])
                    P.op("act", lambda e: e.activation(out=sm[:, 0, :], in_=sm[:, 0, :], func=AF.Exp), r=[sm], w=[sm])
                    P.op("act", lambda e: e.activation(out=sm[:, 1, :], in_=sm[:, 0, :], func=AF.Ln, bias=1.0), r=[sm], w=[sm])
                    P.op("dve", lambda e: e.tensor_tensor(out=sm[:, 2, :], in0=sm[:, 1, :], in1=small[:, 16:32], op=ALU.mult),
                         r=[sm, small], w=[sm])
                    P.op("pe", lambda e: e.matmul(ps[0][:, 16:32], lhsT=tri[:], rhs=sm[:, 2, :], start=True, stop=True),
                         r=[tri, sm], w=[ps[0]])
                    P.op("pe", lambda e: e.matmul(ps[0][:, 32:48], lhsT=ones_f[:], rhs=sm[:, 2, :], start=True, stop=True),
                         r=[ones_f, sm], w=[ps[0]])
                    P.op("act", lambda e: e.copy(out=sm[:, 3:5, :].rearrange("p a b -> p (a b)"), in_=ps[0][:, 16:48]), r=[ps[0]], w=[sm])
                    P.op("act", lambda e: e.activation(out=sm[:, 5, :], in_=sm[:, 3, :], func=AF.Exp), r=[sm], w=[sm])
                    P.op("dve", lambda e: e.tensor_tensor(out=sm[:, 6, :], in0=sm[:, 4, :], in1=sm[:, 3, :], op=ALU.subtract), r=[sm], w=[sm])
                    P.op("act", lambda e: e.activation(out=sm[:, 6, :], in_=sm[:, 6, :], func=AF.Exp), r=[sm], w=[sm])
                    P.op("act", lambda e: e.activation(out=sm[:, 7, :], in_=sm[:, 4, :], func=AF.Exp), r=[sm], w=[sm])
                    P.op("dve", lambda e: e.tensor_tensor(out=sm2[:], in0=sm[:, 1, :], in1=sm[:, 6, :], op=ALU.mult), r=[sm], w=[sm2])
                    if full:
                        P.op("pool", lambda e: e.tensor_tensor(out=xdt[:], in0=xs_tok[:], in1=bc(sm[:, 1, :].unsqueeze(2), [128, 16, 64]),
                                                               op=ALU.mult), r=[xs_tok, sm], w=[xdt])
                    P.op("pool", lambda e: e.tensor_tensor(out=xdte[:], in0=xs_tok[:], in1=bc(sm2[:].unsqueeze(2), [128, 16, 64]),
                                                           op=ALU.mult), r=[xs_tok, sm2], w=[xdte])
                    if full:
                        for g in range(2):
                            P.op("pe", lambda e, g=g: e.matmul(ps[5][:, 256 + g * 128:384 + g * 128], lhsT=bct[:, g, :], rhs=bct[:, 2 + g, :],
                                                               start=True, stop=True), r=[bct], w=[ps[5]])
                        P.op("dve", lambda e: e.tensor_tensor(out=CBm[:], in0=ps[5][:, 256:512].rearrange("p (g t) -> p g t", g=2),
                                                              in1=bc(tri[:].unsqueeze(1), [128, 2, 128]), op=ALU.mult),
                             r=[ps[5], tri], w=[CBm])
                        t1 = f32a
                        for g in range(2):
                            pb = ps[5 + g]
                            P.op("pe", lambda e, g=g, pb=pb: e.matmul(pb[:], lhsT=bct[:, 2 + g, :], rhs=H_bf[:, g * 512:(g + 1) * 512],
                                                                      start=True, stop=True), r=[bct, H_bf], w=[pb])
                            P.op("dve", lambda e, g=g, pb=pb: e.tensor_tensor(out=t1[:, g * 512:(g + 1) * 512].rearrange("p (a b) -> p a b", a=8),
                                                                              in0=pb[:].rearrange("p (a b) -> p a b", a=8),
                                                                              in1=bc(sm[:, 5, g * 8:(g + 1) * 8].unsqueeze(2), [128, 8, 64]),
                                                                              op=ALU.mult), r=[pb, sm], w=[t1])
                        for q4 in range(4):
                            g = q4 // 2
                            Rb, Db, Mb = Rq[q4 % 2], Dq[q4 % 2], MTq[q4 % 2]
                            pseg = ps[5 + q4 % 2]
                            pyd = ps[3 + g]
                            P.op("dve", lambda e, q4=q4, Rb=Rb: e.tensor_tensor(out=Rb[:], in0=bc(tri[:].unsqueeze(1), [128, 4, 128]),
                                                                               in1=bc(sm[:, 2, 4 * q4:4 * q4 + 4].unsqueeze(2), [128, 4, 128]),
                                                                               op=ALU.mult), r=[tri, sm], w=[Rb])
                            P.op("pe", lambda e, Rb=Rb, pseg=pseg: e.matmul(pseg[:], lhsT=su[:], rhs=Rb[:].rearrange("p a b -> p (a b)"),
                                                                           start=True, stop=True), r=[su, Rb], w=[pseg])
                            P.op("act", lambda e, Db=Db, pseg=pseg: e.activation(out=Db[:].rearrange("p a b -> p (a b)"), in_=pseg[:], func=AF.Exp),
                                 r=[pseg], w=[Db])
                            P.op("pool", lambda e, Db=Db, Mb=Mb, g=g: e.tensor_tensor(out=Mb[:], in0=Db[:], in1=bc(CBm[:, g, :].unsqueeze(1), [128, 4, 128]),
                                                                                     op=ALU.mult), r=[Db, CBm], w=[Mb])
                            for hh in range(4):
                                h = 4 * q4 + hh
                                P.op("pe", lambda e, h=h, hh=hh, Mb=Mb, pyd=pyd: e.matmul(pyd[:, (h % 8) * 64:(h % 8 + 1) * 64], lhsT=Mb[:, hh, :],
                                                                                       rhs=xdt[:, h, :], start=True, stop=True), r=[Mb, xdt], w=[pyd])
                    for g in range(2):
                        pb = ps[5 + g]
                        P.op("pe", lambda e, g=g, pb=pb: e.matmul(pb[:], lhsT=B_tok[:, g * 128:(g + 1) * 128],
                                                                  rhs=xdte[:, g * 8:(g + 1) * 8, :].rearrange("p a b -> p (a b)"),
                                                                  start=True, stop=True), r=[B_tok, xdte], w=[pb])
                    P.op("dve", lambda e: e.tensor_tensor(out=H[:], in0=H[:], in1=bc(sm[:, 7, :].unsqueeze(2), [128, 16, 64]), op=ALU.mult),
                         r=[H, sm], w=[H])
                    for g in range(2):
                        pb = ps[5 + g]
                        P.op("dve", lambda e, g=g, pb=pb: e.tensor_tensor(out=H[:, g * 8:(g + 1) * 8, :], in0=H[:, g * 8:(g + 1) * 8, :],
                                                                          in1=pb[:].rearrange("p (a b) -> p a b", a=8), op=ALU.add),
                             r=[H, pb], w=[H])
                    P.op("pool", lambda e: e.tensor_copy(out=H_bf[:], in_=H[:].rearrange("p a b -> p (a b)")), r=[H], w=[H_bf])
                    if not full:
                        return
                    t1 = f32a
                    for g in range(2):
                        P.op("dve", lambda e, g=g: e.tensor_tensor(out=t1[:, g * 512:(g + 1) * 512], in0=t1[:, g * 512:(g + 1) * 512],
                                                                   in1=ps[3 + g][:], op=ALU.add), r=[t1, ps[3 + g]], w=[t1])
                    P.op("pool", lambda e: e.tensor_tensor(out=f32b[:].rearrange("p (a b) -> p a b", a=16), in0=xs_tok[:],
                                                           in1=bc(small[:, 32:48].unsqueeze(2), [128, 16, 64]), op=ALU.mult),
                         r=[xs_tok, small], w=[f32b])
                    P.op("pool", lambda e: e.tensor_tensor(out=t1[:], in0=t1[:], in1=f32b[:], op=ALU.add), r=[t1, f32b], w=[t1])
                    for g in range(2):
                        P.op("act", lambda e, g=g: e.activation(out=f32b[:, g * 512:(g + 1) * 512], in_=ps[1 + g][:], func=AF.Silu),
                             r=[ps[1 + g]], w=[f32b])
                    P.op("dve", lambda e: e.tensor_tensor(out=t1[:], in0=t1[:], in1=f32b[:], op=ALU.mult), r=[t1, f32b], w=[t1])
                    P.op("act", lambda e: e.activation(out=junk_bf[:], in_=t1[:], func=AF.Square, accum_out=st1[:, 2:3]),
                         r=[t1], w=[junk_bf, st1])
                    P.op("act", lambda e: e.activation(out=st1[:, 3:4], in_=st1[:, 2:3], func=AF.Sqrt, scale=1.0 / D, bias=EPS),
                         r=[st1], w=[st1])
                    P.op("dve", lambda e: e.reciprocal(out=st1[:, 3:4], in_=st1[:, 3:4]), r=[st1], w=[st1])
                    P.op("dve", lambda e: e.scalar_tensor_tensor(out=yn_bf[:], in0=t1[:], scalar=st1[:, 3:4], in1=ssdn_b[:],
                                                                 op0=ALU.mult, op1=ALU.mult), r=[t1, st1, ssdn_b], w=[yn_bf])
                    for k in range(8):
                        P.op("pe", lambda e, k=k: e.transpose(out=ps_bf[:, k * 128:(k + 1) * 128], in_=yn_bf[:, k * 128:(k + 1) * 128],
                                                              identity=ident_bf[:]), r=[yn_bf, ident_bf], w=[ps_bf])
                    P.op("act", lambda e: e.copy(out=ynT[:].rearrange("p k t -> p (k t)"), in_=ps_bf[:]), r=[ps_bf], w=[ynT])
                    for cb in range(2):
                        for kc in range(12):
                            lt = ypT[:, kc, :] if kc < 4 else ynT[:, kc - 4, :]
                            P.op("pe", lambda e, kc=kc, cb=cb, lt=lt: e.matmul(ps[1 + cb][:], lhsT=lt, rhs=w_out[:, kc, cb * 512:(cb + 1) * 512],
                                                                              start=(kc == 0), stop=(kc == 11)),
                                 r=[ypT, ynT, w_out], w=[ps[1 + cb]])
                        P.op("dve", lambda e, cb=cb: e.tensor_tensor(out=x_t[:, cb * 512:(cb + 1) * 512], in0=x_t[:, cb * 512:(cb + 1) * 512],
                                                                     in1=ps[1 + cb][:], op=ALU.add), r=[x_t, ps[1 + cb]], w=[x_t])
                    P.dma("sp", lambda e: e.dma_start(out=dst_ap, in_=x_t[:]), r=[x_t], w=[dst_b])

                maskT = None
                if cfg["own"] == "xs2sel":
                    maskT = P.sb("maskT", [128, 1024], mybir.dt.uint8, sc)
                    P.op("dve", lambda e: e.tensor_copy(out=maskT[:], in_=bc(flag[:, 0:1], [128, 1024])), r=[flag], w=[maskT])
                pre_b = xs2_b if cfg["pre"] == "xs2" else None
                for ti in range(NP):
                    mix_tile(ti, (src_pre[ti * 128:(ti + 1) * 128, :], pre_b), None, None, False, 1 if ti == 0 else 0, False, ti == NP - 1)

                def own_src(ti):
                    rr = slice(ti * 128, (ti + 1) * 128)
                    if cfg["own"] == "x_own":
                        return (x_own[rr, :], None)
                    if cfg["own"] == "xs2":
                        return (xs2[rr, :], xs2_b)
                    assert cfg["own"] == "xs2sel"
                    return (xs2[rr, :], xs2[NO * 128 + ti * 128:NO * 128 + (ti + 1) * 128, :], xs2_b)

                for ti in range(NO):
                    mix_tile(NP + ti, own_src(ti),
                             (x_out if stages == "mix" else xs1)[ti * 128:(ti + 1) * 128, :], xout_b if stages == "mix" else xs1_b, True,
                             (cfg["own0_sel"] if ti == 0 else 0), (ti == 0 and cfg["flag"]), True)
                P.barrier()

            if stages == "mix":
                continue
            with contextlib.ExitStack() as sc:
                NG = 16
                w_q = P.sb("w_q_bf", [128, 8, 2048], BF16, sc)
                keysT = P.sb("keysT_bf", [128, 16, 128], BF16, sc)
                gf_b = P.sb("gf_b", [128, 1024], F32, sc)
                shf_b = P.sb("shf_b", [128, 1024], F32, sc)
                gatef_b = P.sb("gatef_b", [128, 1024], F32, sc)
                nfin_b = P.sb("nfin_b", [128, 1024], F32, sc) if (last and final_norm) else None
                xp = [P.sb("xp%d" % i, [128, 1024], F32, sc) for i in range(2)]
                hf = [P.sb("hf%d" % i, [128, 1024], F32, sc) for i in range(2)]
                f32c = P.sb("f32c", [128, 1024], F32, sc)
                junk2 = P.sb("junk2", [128, 1024], BF16, sc)
                acc = P.sb("acc", [128, 1024], F32, sc)
                junk_bf = P.sb("junk_bf2", [128, 1024], BF16, sc)
                hf_bf = P.sb("hf_bf", [128, 1024], BF16, sc)
                hfT = P.sb("hfT", [128, 8, 128], BF16, sc)
                qT = P.sb("qT", [128, 16, 128], BF16, sc)
                scs = P.sb("scs", [128, 16, 128], F32, sc)
                wk = P.sb("wk", [128, 128], F32, sc)
                wk2 = P.sb("wk2", [128, 256], F32, sc)
                v = P.sb("v", [128, 8, 2, 16], F32, sc)
                iu = P.sb("iu", [128, 8, 2, 16], U32, sc)
                iff = P.sb("iff", [128, 8, 2, 16], F32, sc)
                cand = P.sb("cand", [128, 8, 16, 16], F32, sc)
                cidx = P.sb("cidx", [128, 8, 16, 16], F32, sc)
                tops = P.sb("tops", [128, 8, 16], F32, sc)
                gex = P.sb("gex", [128, 8, 16], F32, sc)
                gz = P.sb("gz", [128, 16], F32, sc)
                gates = [P.sb("gates%d" % i, [128, 128], F32, sc) for i in range(2)]
                idxf = P.sb("idxf", [128, 128], F32, sc)
                idxu = [P.sb("idxu%d" % i, [128, 128], U32, sc) for i in range(2)]
                av = P.sb("av", [128, 128], F32, sc)
                wgt = P.sb("wgt", [128, 128], F32, sc)
                st2 = P.sb("st2", [128, 4], F32, sc)
                ring = [P.sb("ring%d" % i, [128, 1024], BF16, sc) for i in range(NG)]
                diag = [P.sb("diag%d" % i, [128, 128], BF16, sc) for i in range(4)]
                rstate = {"n": 0}

                P.dma("sp", lambda e: e.dma_start(out=gf_b[:], in_=modb[:, 4096:5120]), r=[modb_b], w=[gf_b])
                P.dma("sp", lambda e: e.dma_start(out=f32c[:], in_=W["nffn_b"][:, :]), w=[f32c])
                P.op("dve", lambda e: e.scalar_tensor_tensor(out=gf_b[:], in0=gf_b[:], scalar=1.0, in1=f32c[:],
                                                             op0=ALU.add, op1=ALU.mult), r=[gf_b, f32c], w=[gf_b])
                P.dma("sp", lambda e: e.dma_start(out=shf_b[:], in_=modb[:, 3072:4096]), r=[modb_b], w=[shf_b])
                P.dma("sp", lambda e: e.dma_start(out=gatef_b[:], in_=modb[:, 5120:6144]), r=[modb_b], w=[gatef_b])
                if nfin_b is not None:
                    P.dma("sp", lambda e: e.dma_start(out=nfin_b[:], in_=nfin_d[:, :]), w=[nfin_b])
                stg = [xp[0], xp[1], hf[0], hf[1]]
                wqv = W["w_q"].rearrange("(k p) n -> p k n", p=128)
                i = 0
                for k in range(8):
                    for c0 in range(0, 2048, 1024):
                        sT = stg[i % 4]
                        q = ["sp", "act"][i % 2]
                        P.dma(q, lambda e, sT=sT, k=k, c0=c0: e.dma_start(out=sT[:], in_=wqv[:, k, c0:c0 + 1024]), w=[sT])
                        ce = ["dve", "pool"][i % 2]
                        P.op(ce, lambda e, sT=sT, k=k, c0=c0: e.tensor_copy(out=w_q[:, k, c0:c0 + 1024], in_=sT[:]), r=[sT], w=[w_q])
                        i += 1
                for c0 in range(0, 2048, 1024):
                    sT = stg[i % 4]
                    P.dma("sp", lambda e, sT=sT, c0=c0: e.dma_start(out=sT[:], in_=W["keysT"][:, c0:c0 + 1024]), w=[sT])
                    P.op("dve", lambda e, sT=sT, c0=c0: e.tensor_copy(out=keysT[:].rearrange("p b n -> p (b n)")[:, c0:c0 + 1024], in_=sT[:]),
                         r=[sT], w=[keysT])
                    i += 1

                def peer_A(ti):
                    b = ti % 2
                    x_t, hfx = xp[b], hf[b]
                    P.dma("sp", lambda e: e.dma_start(out=x_t[:], in_=xs1[ti * 128:(ti + 1) * 128, :]), r=[xs1_b], w=[x_t])
                    P.op("act", lambda e: e.activation(out=junk_bf[:], in_=x_t[:], func=AF.Square, accum_out=st2[:, 0:1]),
                         r=[x_t], w=[junk_bf, st2])
                    P.op("act", lambda e: e.activation(out=st2[:, 1:2], in_=st2[:, 0:1], func=AF.Sqrt, scale=1.0 / D, bias=EPS),
                         r=[st2], w=[st2])
                    P.op("dve", lambda e: e.reciprocal(out=st2[:, 1:2], in_=st2[:, 1:2]), r=[st2], w=[st2])
                    P.op("dve", lambda e: e.scalar_tensor_tensor(out=f32c[:], in0=x_t[:], scalar=st2[:, 1:2], in1=gf_b[:],
                                                                 op0=ALU.mult, op1=ALU.mult), r=[x_t, st2, gf_b], w=[f32c])
                    P.op("dve", lambda e: e.tensor_tensor(out=pshf[:], in0=f32c[:], in1=shf_b[:], op=ALU.add), r=[f32c, shf_b], w=[pshf])
                    P.op("act", lambda e: e.copy(out=hf_bf[:], in_=pshf[:]), r=[pshf], w=[hf_bf])
                    for k in range(8):
                        P.op("pe", lambda e, k=k: e.transpose(out=ps_bf[:, k * 128:(k + 1) * 128], in_=hf_bf[:, k * 128:(k + 1) * 128],
                                                              identity=ident_bf[:]), r=[hf_bf, ident_bf], w=[ps_bf])
                    P.op("act", lambda e: e.copy(out=hfT[:].rearrange("p k t -> p (k t)"), in_=ps_bf[:]), r=[ps_bf], w=[hfT])
                    for gq in range(4):
                        pb = ps[gq % 2]
                        for jj in range(4):
                            blk = gq * 4 + jj
                            for k in range(8):
                                P.op("pe", lambda e, k=k, jj=jj, blk=blk, pb=pb: e.matmul(pb[:, jj * 128:(jj + 1) * 128],
                                                                                          lhsT=w_q[:, k, blk * 128:(blk + 1) * 128], rhs=hfT[:, k, :],
                                                                                          start=(k == 0), stop=(k == 7)), r=[w_q, hfT], w=[pb])
                        P.op("act", lambda e, gq=gq, pb=pb: e.copy(out=qT[:, gq * 4:(gq + 1) * 4, :].rearrange("p a b -> p (a b)"), in_=pb[:]),
                             r=[pb], w=[qT])
                    for gq in range(4):
                        pb = ps[gq % 2]
                        for jj in range(4):
                            blk = gq * 4 + jj
                            P.op("pe", lambda e, jj=jj, blk=blk, pb=pb: e.matmul(pb[:, jj * 128:(jj + 1) * 128], lhsT=qT[:, blk, :],
                                                                                 rhs=keysT[:, blk, :], start=True, stop=True),
                                 r=[qT, keysT], w=[pb])
                        P.op("act", lambda e, gq=gq, pb=pb: e.copy(out=scs[:, gq * 4:(gq + 1) * 4, :].rearrange("p a b -> p (a b)"), in_=pb[:]),
                             r=[pb], w=[scs])
                    for blk in range(16):
                        h, s = blk // 2, blk % 2
                        P.op("dve", lambda e, blk=blk, h=h, s=s: e.max(out=v[:, h, s, 0:8], in_=scs[:, blk, :]), r=[scs], w=[v])
                        P.op("dve", lambda e, blk=blk, h=h, s=s: e.match_replace(out=wk[:], in_to_replace=v[:, h, s, 0:8],
                                                                                 in_values=scs[:, blk, :], imm_value=NEG), r=[scs, v], w=[wk])
                        P.op("dve", lambda e, h=h, s=s: e.max(out=v[:, h, s, 8:16], in_=wk[:]), r=[wk], w=[v])
                        P.op("dve", lambda e, blk=blk, h=h, s=s: e.max_index(out=iu[:, h, s, 0:8], in_max=v[:, h, s, 0:8],
                                                                             in_values=scs[:, blk, :]), r=[scs, v], w=[iu])
                        P.op("dve", lambda e, blk=blk, h=h, s=s: e.max_index(out=iu[:, h, s, 8:16], in_max=v[:, h, s, 8:16],
                                                                             in_values=scs[:, blk, :]), r=[scs, v], w=[iu])
                    P.op("dve", lambda e: e.tensor_copy(out=iff[:], in_=iu[:]), r=[iu], w=[iff])
                    P.op("dve", lambda e: e.tensor_scalar(out=iff[:, :, 0, :], in0=iff[:, :, 0, :], scalar1=128.0, scalar2=None, op0=ALU.mult),
                         r=[iff], w=[iff])
                    P.op("dve", lambda e: e.tensor_tensor(out=cand[:], in0=bc(v[:, :, 0, :].unsqueeze(3), [128, 8, 16, 16]),
                                                          in1=bc(v[:, :, 1, :].unsqueeze(2), [128, 8, 16, 16]), op=ALU.add), r=[v], w=[cand])
                    P.op("dve", lambda e: e.tensor_tensor(out=cidx[:], in0=bc(iff[:, :, 0, :].unsqueeze(3), [128, 8, 16, 16]),
                                                           in1=bc(iff[:, :, 1, :].unsqueeze(2), [128, 8, 16, 16]), op=ALU.add), r=[iff], w=[cidx])
                    for h in range(8):
                        ch = cand[:, h, :, :].rearrange("p a b -> p (a b)")
                        P.op("dve", lambda e, h=h, ch=ch: e.max(out=tops[:, h, 0:8], in_=ch), r=[cand], w=[tops])
                        P.op("dve", lambda e, h=h, ch=ch: e.match_replace(out=wk2[:], in_to_replace=tops[:, h, 0:8], in_values=ch, imm_value=NEG),
                             r=[cand, tops], w=[wk2])
                        P.op("dve", lambda e, h=h: e.max(out=tops[:, h, 8:16], in_=wk2[:]), r=[wk2], w=[tops])
                    for h in range(8):
                        ch = cand[:, h, :, :].rearrange("p a b -> p (a b)")
                        ci = cidx[:, h, :, :].rearrange("p a b -> p (a b)")
                        for k in range(16):
                            P.op("dve", lambda e, h=h, k=k, ch=ch, ci=ci: e.scalar_tensor_tensor(out=wk2[:], in0=ch, scalar=tops[:, h, k:k + 1], in1=ci,
                                                                                              op0=ALU.is_equal, op1=ALU.mult,
                                                                                              accum_out=idxf[:, h * 16 + k:h * 16 + k + 1]),
                                 r=[cand, cidx, tops], w=[wk2, idxf])
                    P.op("dve", lambda e: e.tensor_scalar(out=idxf[:], in0=idxf[:], scalar1=16383.0, scalar2=0.0, op0=ALU.min, op1=ALU.max),
                         r=[idxf], w=[idxf])
                    P.op("dve", lambda e: e.tensor_copy(out=idxu[b][:], in_=idxf[:]), r=[idxf], w=[idxu[b]])

                def peer_A2(ti):
                    b = ti % 2
                    P.op("dve", lambda e: e.tensor_tensor(out=gex[:], in0=tops[:], in1=bc(tops[:, :, 0:1], [128, 8, 16]), op=ALU.subtract),
                         r=[tops], w=[gex])
                    P.op("act", lambda e: e.activation(out=gex[:], in_=gex[:], func=AF.Exp), r=[gex], w=[gex])
                    P.op("dve", lambda e: e.tensor_reduce(out=gz[:, 0:8], in_=gex[:], axis=mybir.AxisListType.X, op=ALU.add), r=[gex], w=[gz])
                    P.op("dve", lambda e: e.reciprocal(out=gz[:, 8:16], in_=gz[:, 0:8]), r=[gz], w=[gz])
                    P.op("dve", lambda e: e.tensor_tensor(out=gates[b][:].rearrange("p (h k) -> p h k", h=8), in0=gex[:],
                                                          in1=bc(gz[:, 8:16].unsqueeze(2), [128, 8, 16]), op=ALU.mult), r=[gex, gz], w=[gates[b]])

                def gather(tabk, col, idxT):
                    tab, tabb = TB[(l, tabk)]
                    n = rstate["n"]
                    if n % 4 == 0:
                        P._wait("pool", [], [ring[(n + k) % NG] for k in range(4)])
                    slot = ring[n % NG]
                    rstate["n"] += 1
                    P.dma("pool", lambda e: e.indirect_dma_start(out=slot[:], out_offset=None, in_=tab[:, :],
                                                                 in_offset=bass.IndirectOffsetOnAxis(ap=idxT[:, col:col + 1], axis=0)),
                          r=[idxT, tabb], w=[slot])
                    return slot

                def peer_B(ti):
                    b = ti % 2
                    x_t, hfx = xp[b], hf[b]
                    LOOK = NG - 4
                    order = [("d", j) for j in range(128)] + [("u", j) for j in range(128)]
                    slots = {}
                    issued = 0

                    def issue_upto(n):
                        nonlocal issued
                        while issued < min(n, len(order)):
                            kind, j = order[issued]
                            slots[issued] = gather("e_down" if kind == "d" else "e_up", j, idxu[b])
                            issued += 1

                    for pos, (kind, j) in enumerate(order):
                        if pos == 128 and ti + 1 < NO:
                            peer_A(ti + 1)
                        issue_upto(pos + LOOK)
                        sl = slots.pop(pos)
                        if kind == "d":
                            P.op("dve", lambda e, sl=sl, j=j: e.scalar_tensor_tensor(out=junk2[:], in0=sl[:], scalar=1.0, in1=pshf[:],
                                                                                     op0=ALU.mult, op1=ALU.mult, accum_out=av[:, j:j + 1]),
                                 r=[sl, pshf], w=[junk2, av])
                            if j == 127:
                                P.op("act", lambda e: e.activation(out=wgt[:], in_=av[:], func=AF.Gelu), r=[av], w=[wgt])
                                P.op("dve", lambda e: e.tensor_tensor(out=wgt[:], in0=wgt[:], in1=gates[b][:], op=ALU.mult),
                                     r=[wgt, gates[b]], w=[wgt])
                        else:
                            dg = diag[j % 4]
                            P.op("act", lambda e, dg=dg, j=j: e.activation(out=dg[:], in_=ident_f[:], func=AF.Copy, scale=wgt[:, j:j + 1]),
                                 r=[ident_f, wgt], w=[dg])
                            for hh in range(2):
                                P.op("pe", lambda e, dg=dg, sl=sl, j=j, hh=hh: e.matmul(ps[4 + hh][:], lhsT=dg[:],
                                                                                     rhs=sl[:, hh * 512:(hh + 1) * 512],
                                                                                     start=(j == 0), stop=(j == 127)),
                                     r=[dg, sl], w=[ps[4 + hh]])
                    for hh in range(2):
                        P.op("dve", lambda e, hh=hh: e.tensor_tensor(out=acc[:, hh * 512:(hh + 1) * 512], in0=ps[4 + hh][:],
                                                                     in1=gatef_b[:, hh * 512:(hh + 1) * 512], op=ALU.mult),
                             r=[ps[4 + hh], gatef_b], w=[acc])
                    P.op("dve", lambda e: e.tensor_tensor(out=x_t[:], in0=x_t[:], in1=acc[:], op=ALU.add), r=[x_t, acc], w=[x_t])
                    if last and final_norm:
                        P.op("act", lambda e: e.activation(out=junk_bf[:], in_=x_t[:], func=AF.Square, accum_out=st2[:, 2:3]),
                             r=[x_t], w=[junk_bf, st2])
                        P.op("act", lambda e: e.activation(out=st2[:, 3:4], in_=st2[:, 2:3], func=AF.Sqrt, scale=1.0 / D, bias=EPS),
                             r=[st2], w=[st2])
                        P.op("dve", lambda e: e.reciprocal(out=st2[:, 3:4], in_=st2[:, 3:4]), r=[st2], w=[st2])
                        P.op("dve", lambda e: e.scalar_tensor_tensor(out=x_t[:], in0=x_t[:], scalar=st2[:, 3:4], in1=nfin_b[:],
                                                                     op0=ALU.mult, op1=ALU.mult), r=[x_t, st2, nfin_b], w=[x_t])
                    dstb = xout_b if last else xs2_b
                    P.dma("sp", lambda e: e.dma_start(out=dst_final[ti * 128:(ti + 1) * 128, :], in_=x_t[:]), r=[x_t], w=[dstb])

                peer_A(0)
                peer_A2(0)
                for ti in range(NO):
                    peer_B(ti)
                    if ti + 1 < NO:
                        peer_A2(ti + 1)
                P.barrier()
        P.barrier(["sp"])
    return nc


def _layer_inputs(l, p):
    f = np.float32
    out = {}
    out["w_ada"] = np.ascontiguousarray(p["w_ada"][l], f)
    out["b_ada"] = np.ascontiguousarray(p["b_ada"][l][None, :], f)
    rep = lambda vec: np.ascontiguousarray(np.broadcast_to(np.asarray(vec, f)[None, :], (128, vec.shape[0])))
    out["nmix_b"] = rep(p["norm_mix"][l])
    out["nffn_b"] = rep(p["norm_ffn"][l])
    out["ssdn_b"] = rep(p["ssd_norm"][l])
    out["w_in"] = np.ascontiguousarray(p["w_in"][l], f)
    out["w_out"] = np.ascontiguousarray(p["w_out"][l], f)
    out["w_q"] = np.ascontiguousarray(p["w_query"][l], f)
    out["pool_w"] = np.ascontiguousarray(p["pool_w"][l], f)
    out["pool_sb"] = np.ascontiguousarray(np.concatenate([p["pool_scale"][l].T, p["pool_b"][l].T], axis=1), f)
    cw = p["conv_w"][l].T.reshape(12, 128, 4).transpose(1, 0, 2)
    cb = p["conv_b"][l].reshape(12, 128).T[:, :, None]
    out["conv_wb"] = np.ascontiguousarray(np.concatenate([cw, cb], axis=2).reshape(128, 60), f)
    out["ssd_small"] = rep(np.concatenate([p["dt_bias"][l], p["a_log"][l], p["d_skip"][l]]))
    k = np.stack([p["sub_keys1"][l], p["sub_keys2"][l]], axis=1)
    out["keysT"] = np.ascontiguousarray(k.transpose(3, 0, 1, 2).reshape(128, 2048), f)
    out["e_down"] = np.ascontiguousarray(p["expert_down"][l], f)
    out["e_up"] = np.ascontiguousarray(p["expert_up"][l], f)
    return out


def _invc_tables():
    t = np.arange(128)
    const = np.stack([np.full(128, 1.0 / w) for w in (2, 4, 8, 16)])
    start = np.stack([1.0 / np.minimum(t + 1, w) for w in (2, 4, 8, 16)])
    return const.astype(np.float32), start.astype(np.float32)


def _core_common(c_row, first_half):
    const, start = _invc_tables()
    own0 = start if first_half else const
    invc = np.concatenate([const.reshape(-1), start.reshape(-1), own0.reshape(-1)])
    return {
        "flag": np.full((128, 1), 0.0 if first_half else 1.0, np.float32),
        "invc": np.ascontiguousarray(np.broadcast_to(invc[None, :], (128, 1536)), np.float32),
        "cT": np.ascontiguousarray(c_row.reshape(8, 128).T, np.float32),
    }


_NC_CACHE = {}
FUSED = True


def _get_nc(key, cfgs):
    if key not in _NC_CACHE:
        _NC_CACHE[key] = build_program(cfgs)
    return _NC_CACHE[key]


def kernel(**p):
    p = {k: np.asarray(v) for k, v in p.items()}
    x = p["x"].astype(np.float32)
    Bsz, S, _ = x.shape
    HALF = S // 2
    NT = HALF // 128
    ncores = 2 * Bsz
    nfin = np.ascontiguousarray(np.broadcast_to(p["norm_final"].astype(np.float32)[None, :], (128, D)))
    if FUSED:
        cfgs = [dict(l="0", NP=0, NO=2 * NT, pre=None, own="x_own", flag=False, own0_sel=1, dst="xs2", final_norm=False),
                dict(l="1", NP=NT, NO=NT, pre="xs2", own="xs2sel", flag=True, own0_sel=2, dst="x_out", final_norm=True)]
        nc = _get_nc(("fused", NT), cfgs)
        lw = {}
        for l in range(2):
            lw.update({k + "_%d" % l: v for k, v in _layer_inputs(l, p).items()})
        in_maps = []
        for core in range(ncores):
            b, hh = core // 2, core % 2
            m = dict(lw)
            m.update(_core_common(p["c"][b].astype(np.float32), hh == 0))
            m["x_own"] = np.ascontiguousarray(x[b])
            m["nfin_b"] = nfin
            in_maps.append(m)
        res = run_bass_kernel_spmd(nc, in_maps, core_ids=list(range(ncores)))
        out = np.empty_like(x)
        for core in range(ncores):
            b, hh = core // 2, core % 2
            out[b, hh * HALF:(hh + 1) * HALF] = res.results[core]["x_out"]
        return out
    cur = x
    for l in range(2):
        last = (l == 1)
        cfgs = [dict(l="0", NP=NT, NO=NT, pre="x_pre", own="x_own", flag=True, own0_sel=2, dst="x_out", final_norm=last)]
        nc = _get_nc(("layer", last, NT), cfgs)
        lw = {k + "_0": v for k, v in _layer_inputs(l, p).items()}
        in_maps = []
        for core in range(ncores):
            b, hh = core // 2, core % 2
            m = dict(lw)
            m.update(_core_common(p["c"][b].astype(np.float32), hh == 0))
            m["x_own"] = np.ascontiguousarray(cur[b, hh * HALF:(hh + 1) * HALF])
            m["x_pre"] = np.ascontiguousarray(cur[b, 0:HALF])
            m["nfin_b"] = nfin
            in_maps.append(m)
        res = run_bass_kernel_spmd(nc, in_maps, core_ids=list(range(ncores)))
        nxt = np.empty_like(cur)
        for core in range(ncores):
            b, hh = core // 2, core % 2
            nxt[b, hh * HALF:(hh + 1) * HALF] = res.results[core]["x_out"]
        cur = nxt
    return cur
```

```python
import contextlib
import numpy as np
import concourse.bass as bass
import concourse.mybir as mybir
from concourse.bass_utils import run_bass_kernel_spmd

F32 = mybir.dt.float32
BF16 = mybir.dt.bfloat16
U32 = mybir.dt.uint32
F32R = mybir.dt.float32r
AF = mybir.ActivationFunctionType
ALU = mybir.AluOpType

D = 1024
NIN = 3088
EPS = 1e-6
NEG = -1.0e30
SEM_CAP = 12000
SAME_ENGINE_WAIT = True


class Buf:
    __slots__ = ("name", "w", "r", "excl", "dsem", "dcnt")

    def __init__(self, name, excl=False):
        self.name = name
        self.w = None
        self.r = {}
        self.excl = excl
        self.dsem = None
        self.dcnt = 0


class T:
    def __init__(self, t, b):
        self.t = t
        self.b = b

    def __getitem__(self, k):
        return self.t[k]


class Prog:
    def __init__(self, nc, es):
        self.nc = nc
        self.es = es
        self.E = {"pe": nc.tensor, "act": nc.scalar, "dve": nc.vector, "pool": nc.gpsimd, "sp": nc.sync}
        self.sem = {}
        self.cnt = {}
        self.waited = {k: {} for k in self.E}
        self.latest = {}
        self.nsem = 0
        for k in self.E:
            self._newsem(k)

    def _mksem(self, name):
        self.nsem += 1
        return self.es.enter_context(self.nc.semaphore("%s_%d" % (name, self.nsem)))

    def _newsem(self, k):
        self.sem[k] = self._mksem("s" + k)
        self.cnt[k] = 0

    def sb(self, name, shape, dt, scope=None):
        self.nsem += 1
        name = "%s_t%d" % (name, self.nsem)
        t = (scope or self.es).enter_context(self.nc.sbuf_tensor(name, list(shape), dt))
        return T(t, Buf(name))

    def ps(self, name, shape, dt):
        t = self.es.enter_context(self.nc.psum_tensor(name, list(shape), dt))
        return T(t, Buf(name, excl=True))

    @staticmethod
    def _b(x):
        return x.b if isinstance(x, T) else x

    def _wait(self, eng, r, w):
        toks = []
        for x in r:
            b = self._b(x)
            if b.w is not None:
                toks.append(b.w)
            if b.excl:
                toks.extend(b.r.values())
        for x in w:
            b = self._b(x)
            if b.w is not None and not b.r:
                toks.append(b.w)
            toks.extend(b.r.values())
        e = self.E[eng]
        wd = self.waited[eng]
        for (s, v) in toks:
            if (not SAME_ENGINE_WAIT or eng == "pe") and s is self.sem.get(eng):
                continue
            if wd.get(id(s), 0) >= v:
                continue
            e.wait_ge(s, v)
            wd[id(s)] = v

    def _commit(self, tok, r, w):
        self.latest[id(tok[0])] = tok
        for x in r:
            b = self._b(x)
            if b.excl:
                b.w = tok
                b.r = {}
            else:
                b.r[id(tok[0])] = tok
        for x in w:
            b = self._b(x)
            b.w = tok
            b.r = {}

    def op(self, eng, fn, r=(), w=()):
        self._wait(eng, r, w)
        inst = fn(self.E[eng])
        if self.cnt[eng] >= SEM_CAP:
            self._newsem(eng)
        self.cnt[eng] += 1
        inst.then_inc(self.sem[eng], 1)
        self._commit((self.sem[eng], self.cnt[eng]), r, w)

    def dma(self, q, fn, r=(), w=()):
        self._wait(q, r, w)
        b = self._b(w[0])
        if b.dsem is None or b.dcnt >= SEM_CAP // 16:
            b.dsem = self._mksem("d")
            b.dcnt = 0
        inst = fn(self.E[q])
        b.dcnt += 1
        inst.then_inc(b.dsem, 16)
        self._commit((b.dsem, 16 * b.dcnt), r, w)

    def barrier(self, engs=None):
        toks = list(self.latest.values())
        for eng in (engs or self.E):
            wd = self.waited[eng]
            for (s, v) in toks:
                if wd.get(id(s), 0) >= v:
                    continue
                self.E[eng].wait_ge(s, v)
                wd[id(s)] = v


def bc(ap, shape):
    return ap.to_broadcast(list(shape))


class LayerW:
    pass


NAMES_L = ["w_ada", "b_ada", "nmix_b", "nffn_b", "ssdn_b", "w_in", "w_out", "w_q", "pool_w", "pool_sb",
           "conv_wb", "ssd_small", "keysT", "e_down", "e_up"]
SHAPES_L = {"w_ada": [1024, 6144], "b_ada": [1, 6144], "nmix_b": [128, 1024], "nffn_b": [128, 1024],
            "ssdn_b": [128, 1024], "w_in": [1024, NIN], "w_out": [1536, 1024], "w_q": [1024, 2048],
            "pool_w": [4, 128, 128], "pool_sb": [128, 8], "conv_wb": [128, 60], "ssd_small": [128, 48],
            "keysT": [128, 2048], "e_down": [16384, 1024], "e_up": [16384, 1024]}


def build_program(cfgs, dbg=None, stages="all"):
    layers = [c["l"] for c in cfgs]
    NOMAX = max(c["NO"] for c in cfgs)
    NOUT = cfgs[-1]["NO"]
    nc = bass.Bass("TRN2", target_bir_lowering=False)
    es = contextlib.ExitStack()
    dr = {}

    def din(name, shape, dt=F32):
        dr[name] = nc.dram_tensor(name, list(shape), dt, kind="ExternalInput").ap()
        return dr[name]

    srcs = set()
    for c in cfgs:
        srcs.add(c["pre"]); srcs.add(c["own"])
    x_pre = din("x_pre", [cfgs[0]["NP"] * 128, D]) if "x_pre" in srcs else None
    x_own = din("x_own", [cfgs[0]["NO"] * 128, D]) if "x_own" in srcs else None
    flag_d = din("flag", [128, 1])
    invc_d = din("invc", [128, 3 * 512])
    cT_d = din("cT", [128, 8])
    nfin_d = din("nfin_b", [128, 1024])
    LW = {}
    for l in layers:
        LW[l] = {n: din(n + "_" + l, SHAPES_L[n]) for n in NAMES_L}
    x_out = nc.dram_tensor("x_out", [NOUT * 128, D], F32, kind="ExternalOutput").ap()
    modb = nc.dram_tensor("modb", [128, 6144], F32, kind="Internal").ap()
    xs1 = nc.dram_tensor("xs1", [NOMAX * 128, D], F32, kind="Internal").ap()
    xs2 = nc.dram_tensor("xs2", [NOMAX * 128, D], F32, kind="Internal").ap()
    dbg_out = {}
    if dbg:
        for n, shp in dbg.items():
            dbg_out[n] = nc.dram_tensor("dbg_" + n, list(shp), F32, kind="ExternalOutput").ap()

    with es:
        P = Prog(nc, es)
        modb_b = Buf("modb")
        xs1_b = Buf("xs1")
        xs2_b = Buf("xs2")
        xout_b = Buf("xout")
        dbg_b = Buf("dbg")

        def dump(name, src_T, src_ap):
            if name in dbg_out:
                P.dma("sp", lambda e: e.dma_start(out=dbg_out[name], in_=src_ap), r=[src_T], w=[dbg_b])

        ident_bf = P.sb("ident_bf", [128, 128], BF16)
        ident_f = P.sb("ident_f", [128, 128], F32)
        tri = P.sb("tri", [128, 128], F32)
        su = P.sb("su", [128, 128], F32)
        ones_f = P.sb("ones_f", [128, 128], F32)
        flag = P.sb("flag_sb", [128, 1], F32)
        invc = P.sb("invc_sb", [128, 3, 4, 128], F32)
        small = P.sb("ssd_small_sb", [128, 48], F32)
        conv_wb = P.sb("conv_wb_sb", [128, 12, 5], F32)
        pool_sb = P.sb("pool_sb_sb", [128, 8], F32)
        ps = [P.ps("psb%d" % i, [128, 512], F32) if i not in (2, 3) else None for i in range(7)]
        psA = es.enter_context(nc.psum_tensor("psA2", [128, 1024], F32))
        ps[2] = T(psA[:, 0:512], Buf("psA_lo", excl=True))
        ps[3] = T(psA[:, 512:1024], Buf("psA_hi", excl=True))
        pshf = T(psA[:, :], Buf("pshf", excl=True))
        ps_bf = P.ps("psbf", [128, 1024], BF16)

        P.op("pool", lambda e: e.memset(ones_f[:], 1.0), w=[ones_f])
        P.op("pool", lambda e: e.affine_select(out=ident_f[:], in_=ones_f[:], pattern=[[-1, 128]],
                                               compare_op=ALU.is_equal, fill=0.0, base=0, channel_multiplier=1),
             r=[ones_f], w=[ident_f])
        P.op("pool", lambda e: e.tensor_copy(out=ident_bf[:], in_=ident_f[:]), r=[ident_f], w=[ident_bf])
        P.op("pool", lambda e: e.affine_select(out=tri[:], in_=ones_f[:], pattern=[[1, 128]],
                                               compare_op=ALU.is_ge, fill=0.0, base=0, channel_multiplier=-1),
             r=[ones_f], w=[tri])
        P.op("pool", lambda e: e.affine_select(out=su[:], in_=ones_f[:], pattern=[[-1, 128]],
                                               compare_op=ALU.is_gt, fill=0.0, base=0, channel_multiplier=1),
             r=[ones_f], w=[su])
        P.dma("sp", lambda e: e.dma_start(out=flag[:], in_=flag_d[:, :]), w=[flag])
        P.dma("sp", lambda e: e.dma_start(out=invc[:].rearrange("p a g t -> p (a g t)"), in_=invc_d[:, :]), w=[invc])

        TB = {}
        tb_list = []
        for l in layers:
            for nm in ("e_down", "e_up"):
                tt = nc.dram_tensor("tb_%s_%s" % (nm, l), [16384, D], BF16, kind="Internal").ap()
                TB[(l, nm)] = (tt, Buf("tb_%s_%s" % (nm, l)))
                tb_list.append((LW[l][nm], tt, TB[(l, nm)][1]))
        def emit_conversion():
            with contextlib.ExitStack() as sc:
                CR = 4
                cst = [P.sb("cvt_in%d" % i, [128, CR, D], F32, sc) for i in range(3)]
                cbf = [P.sb("cvt_out%d" % i, [128, CR, D], BF16, sc) for i in range(3)]
                nchunk = 16384 // (128 * CR)
                it = 0
                for c in range(nchunk):
                    for (src, dstt, dstb) in tb_list:
                        a_in, a_out = cst[it % 3], cbf[it % 3]
                        sv = src.rearrange("(c p r) d -> c p r d", p=128, r=CR)
                        dv = dstt.rearrange("(c p r) d -> c p r d", p=128, r=CR)
                        q = ["sp", "act"][it % 2]
                        P.dma(q, lambda e, a_in=a_in, sv=sv, c=c: e.dma_start(out=a_in[:], in_=sv[c]), w=[a_in])
                        if it % 2 == 0:
                            P.op("dve", lambda e, a_in=a_in, a_out=a_out: e.tensor_copy(out=a_out[:], in_=a_in[:]), r=[a_in], w=[a_out])
                        else:
                            P.op("act", lambda e, a_in=a_in, a_out=a_out: e.copy(out=a_out[:], in_=a_in[:]), r=[a_in], w=[a_out])
                        P.dma(q, lambda e, a_out=a_out, dv=dv, c=c: e.dma_start(out=dv[c], in_=a_out[:]), r=[a_out], w=[dstb])
                        it += 1


        for li, l in enumerate(layers):
            W = LW[l]
            cfg = cfgs[li]
            NP, NO = cfg["NP"], cfg["NO"]
            final_norm = cfg["final_norm"]
            last = (cfg["dst"] == "x_out")
            src_pre = {"x_pre": x_pre, "xs2": xs2, None: None}[cfg["pre"]]
            dst_final = x_out if last else xs2
            with contextlib.ExitStack() as sc:
                cact = P.sb("cact", [128, 8], F32, sc)
                crep = P.sb("crep", [128, 8, 128], F32, sc)
                wst = [P.sb("wada_st%d" % i, [128, 8, 512], F32, sc) for i in range(2)]
                brow = [P.sb("brow%d" % i, [1, 512], F32, sc) for i in range(2)]
                mst = [P.sb("mst%d" % i, [128, 512], F32, sc) for i in range(2)]
                P.dma("sp", lambda e: e.dma_start(out=cact[:], in_=cT_d[:, :]), w=[cact])
                P.op("act", lambda e: e.activation(out=cact[:], in_=cact[:], func=AF.Silu), r=[cact], w=[cact])
                for k in range(8):
                    P.op("dve", lambda e, k=k: e.tensor_copy(out=crep[:, k, :], in_=bc(cact[:, k:k + 1], [128, 128])),
                         r=[cact], w=[crep])
                wv = W["w_ada"].rearrange("(k p) n -> p k n", p=128)
                for cb in range(12):
                    st = wst[cb % 2]
                    br = brow[cb % 2]
                    ms = mst[cb % 2]
                    pb = ps[cb % 2]
                    q = "sp" if cb % 2 == 0 else "act"
                    P.dma(q, lambda e, st=st, cb=cb: e.dma_start(out=st[:], in_=wv[:, :, cb * 512:(cb + 1) * 512]), w=[st])
                    P.dma(q, lambda e, br=br, cb=cb: e.dma_start(out=br[:], in_=W["b_ada"][0:1, cb * 512:(cb + 1) * 512]), w=[br])
                    for k in range(8):
                        P.op("pe", lambda e, k=k, st=st, pb=pb: e.matmul(pb[:], lhsT=crep[:, k, :], rhs=st[:, k, :],
                                                                          start=(k == 0), stop=False),
                             r=[crep, st], w=[pb])
                    P.op("pe", lambda e, br=br, pb=pb: e.matmul(pb[:], lhsT=ones_f[0:1, :], rhs=br[0:1, :],
                                                                 start=False, stop=True), r=[ones_f, br], w=[pb])
                    P.op("act", lambda e, ms=ms, pb=pb: e.copy(out=ms[:], in_=pb[:]), r=[pb], w=[ms])
                    P.dma("sp", lambda e, ms=ms, cb=cb: e.dma_start(out=modb[:, cb * 512:(cb + 1) * 512], in_=ms[:]),
                          r=[ms], w=[modb_b])
                P.dma("sp", lambda e: e.dma_start(out=small[:], in_=W["ssd_small"][:, :]), w=[small])
                P.op("act", lambda e: e.activation(out=small[:, 16:32], in_=small[:, 16:32], func=AF.Exp), r=[small], w=[small])
                P.op("dve", lambda e: e.tensor_scalar(out=small[:, 16:32], in0=small[:, 16:32], scalar1=-1.0, scalar2=None,
                                                      op0=ALU.mult), r=[small], w=[small])
                P.dma("sp", lambda e: e.dma_start(out=conv_wb[:].rearrange("p j k -> p (j k)"), in_=W["conv_wb"][:, :]), w=[conv_wb])
                P.dma("sp", lambda e: e.dma_start(out=pool_sb[:], in_=W["pool_sb"][:, :]), w=[pool_sb])
                P.op("dve", lambda e: e.tensor_tensor(out=pool_sb[:, 4:8], in0=pool_sb[:, 4:8], in1=pool_sb[:, 0:4], op=ALU.mult),
                     r=[pool_sb], w=[pool_sb])
                if li == 0:
                    emit_conversion()
                P.barrier()

            with contextlib.ExitStack() as sc:
                w_in = P.sb("w_in_bf", [128, 8, NIN], BF16, sc)
                w_out = P.sb("w_out_bf", [128, 12, 1024], BF16, sc)
                pool_w = P.sb("pool_w_bf", [128, 4, 128], BF16, sc)
                gm_b = P.sb("gm_b", [128, 1024], F32, sc)
                shm_b = P.sb("shm_b", [128, 1024], F32, sc)
                ssdn_b = P.sb("ssdn_b", [128, 1024], F32, sc)
                xt = [P.sb("xt%d" % i, [128, 1024], F32, sc) for i in range(2)]
                f32a = P.sb("f32a", [128, 1024], F32, sc)
                f32b = P.sb("f32b", [128, 1024], F32, sc)
                xs_tok = P.sb("xs_tok", [128, 16, 64], F32, sc)
                H = P.sb("H", [128, 16, 64], F32, sc)
                H_bf = P.sb("H_bf", [128, 1024], BF16, sc)
                junk_bf = P.sb("junk_bf", [128, 1024], BF16, sc)
                h_bf = P.sb("h_bf", [128, 1024], BF16, sc)
                hT = P.sb("hT", [128, 8, 128], BF16, sc)
                u_ext = P.sb("u_ext", [128, 4, 143], F32, sc)
                sA = P.sb("sA", [128, 4, 143], F32, sc)
                sB = P.sb("sB", [128, 4, 143], F32, sc)
                ptmp = P.sb("ptmp", [128, 4, 128], F32, sc)
                mixed = P.sb("mixed", [128, 4, 128], BF16, sc)
                ypT = P.sb("ypT", [128, 4, 128], BF16, sc)
                xbc = P.sb("xbc_ext", [128, 12, 131], F32, sc)
                cacc = P.sb("cacc", [128, 12, 128], F32, sc)
                bct = P.sb("bct", [128, 4, 128], BF16, sc)
                xdt = P.sb("xdt", [128, 16, 64], BF16, sc)
                xdte = P.sb("xdte", [128, 16, 64], BF16, sc)
                B_tok = P.sb("B_tok", [128, 256], BF16, sc)
                sm = P.sb("sm", [128, 8, 16], F32, sc)
                sm2 = P.sb("sm2", [128, 16], F32, sc)
                st1 = P.sb("st1", [128, 4], F32, sc)
                Rq = [P.sb("Rq%d" % i, [128, 4, 128], F32, sc) for i in range(2)]
                Dq = [P.sb("Dq%d" % i, [128, 4, 128], F32, sc) for i in range(2)]
                MTq = [P.sb("MTq%d" % i, [128, 4, 128], BF16, sc) for i in range(2)]
                CBm = P.sb("CBm", [128, 2, 128], F32, sc)
                yn_bf = P.sb("yn_bf", [128, 1024], BF16, sc)
                ynT = P.sb("ynT", [128, 8, 128], BF16, sc)

                P.dma("sp", lambda e: e.dma_start(out=gm_b[:], in_=modb[:, 1024:2048]), r=[modb_b], w=[gm_b])
                P.dma("sp", lambda e: e.dma_start(out=f32a[:], in_=W["nmix_b"][:, :]), w=[f32a])
                P.op("dve", lambda e: e.scalar_tensor_tensor(out=gm_b[:], in0=gm_b[:], scalar=1.0, in1=f32a[:],
                                                             op0=ALU.add, op1=ALU.mult), r=[gm_b, f32a], w=[gm_b])
                P.dma("sp", lambda e: e.dma_start(out=shm_b[:], in_=modb[:, 0:1024]), r=[modb_b], w=[shm_b])
                P.dma("sp", lambda e: e.dma_start(out=ssdn_b[:], in_=W["ssdn_b"][:, :]), w=[ssdn_b])
                gate_m = f32b
                P.dma("sp", lambda e: e.dma_start(out=gate_m[:], in_=modb[:, 2048:3072]), r=[modb_b], w=[gate_m])
                stg = [xt[0], xt[1], f32a, T(xs_tok.t, xs_tok.b)]
                stg_flat = [xt[0][:], xt[1][:], f32a[:], xs_tok[:].rearrange("p a b -> p (a b)")]
                pieces = []
                wiv = W["w_in"].rearrange("(k p) n -> p k n", p=128)
                for k in range(8):
                    for c0 in range(0, NIN, 1024):
                        c1 = min(NIN, c0 + 1024)
                        pieces.append((wiv[:, k, c0:c1], w_in, (k, c0, c1), None))
                wov = W["w_out"].rearrange("(k p) n -> p k n", p=128)
                for k in range(12):
                    pieces.append((wov[:, k, :], w_out, (k, 0, 1024), gate_m))
                cast_engs = ["dve", "pool", "act"]
                for i, (src, dstT, (k, c0, c1), mul) in enumerate(pieces):
                    sT = stg[i % 4]
                    sv = stg_flat[i % 4]
                    n = c1 - c0
                    q = ["sp", "act"][i % 2]
                    P.dma(q, lambda e, sv=sv, src=src, n=n: e.dma_start(out=sv[:, 0:n], in_=src), w=[sT])
                    if mul is None:
                        ce = cast_engs[i % 3]
                        if ce == "act":
                            P.op("act", lambda e, dstT=dstT, k=k, c0=c0, c1=c1, sv=sv, n=n:
                                 e.copy(out=dstT[:, k, c0:c1], in_=sv[:, 0:n]), r=[sT], w=[dstT])
                        else:
                            P.op(ce, lambda e, dstT=dstT, k=k, c0=c0, c1=c1, sv=sv, n=n:
                                 e.tensor_copy(out=dstT[:, k, c0:c1], in_=sv[:, 0:n]), r=[sT], w=[dstT])
                    else:
                        ce = ["dve", "pool"][i % 2]
                        P.op(ce, lambda e, dstT=dstT, k=k, c0=c0, c1=c1, sv=sv, n=n, mul=mul:
                             e.tensor_tensor(out=dstT[:, k, c0:c1], in0=sv[:, 0:n], in1=mul[:, 0:n], op=ALU.mult),
                             r=[sT, mul], w=[dstT])
                P.dma("sp", lambda e: e.dma_start(out=xt[0][:, 0:512].rearrange("p (g d) -> p g d", g=4),
                                                  in_=W["pool_w"].rearrange("g c d -> c g d")), w=[xt[0]])
                P.op("dve", lambda e: e.tensor_copy(out=pool_w[:].rearrange("p g d -> p (g d)"), in_=xt[0][:, 0:512]),
                     r=[xt[0]], w=[pool_w])
                P.op("pool", lambda e: e.memset(H[:], 0.0), w=[H])
                P.op("pool", lambda e: e.memset(H_bf[:], 0.0), w=[H_bf])
                P.op("pool", lambda e: e.memset(u_ext[:], 0.0), w=[u_ext])
                P.op("pool", lambda e: e.memset(xbc[:], 0.0), w=[xbc])

                def mix_tile(ti, src_ap, dst_ap, dst_b, full, invc_sel, apply_flag, need_u):
                    if isinstance(src_ap, tuple) and len(src_ap) == 2:
                        src_ap = [src_ap[0], src_ap[1]]
                    x_t = xt[ti % 2]
                    if apply_flag:
                        P.op("dve", lambda e: e.tensor_scalar(out=H[:], in0=H[:], scalar1=flag[:, 0:1], scalar2=None, op0=ALU.mult),
                             r=[H, flag], w=[H])
                        P.op("pool", lambda e: e.tensor_copy(out=H_bf[:], in_=H[:].rearrange("p a b -> p (a b)")), r=[H], w=[H_bf])
                        P.op("dve", lambda e: e.tensor_scalar(out=u_ext[:, :, 0:15], in0=u_ext[:, :, 0:15], scalar1=flag[:, 0:1],
                                                              scalar2=None, op0=ALU.mult), r=[u_ext, flag], w=[u_ext])
                        P.op("dve", lambda e: e.tensor_scalar(out=xbc[:, :, 0:3], in0=xbc[:, :, 0:3], scalar1=flag[:, 0:1],
                                                              scalar2=None, op0=ALU.mult), r=[xbc, flag], w=[xbc])
                    if isinstance(src_ap, tuple):
                        a_ap, b_ap, rb = src_ap
                        P.dma("sp", lambda e: e.dma_start(out=x_t[:], in_=a_ap), r=[rb], w=[x_t])
                        P.dma("act", lambda e: e.dma_start(out=f32b[:], in_=b_ap), r=[rb], w=[f32b])
                        P.op("dve", lambda e: e.copy_predicated(out=x_t[:], mask=maskT[:], data=f32b[:]), r=[maskT, f32b, x_t], w=[x_t])
                    else:
                        P.dma("sp", lambda e: e.dma_start(out=x_t[:], in_=src_ap[0]), r=[src_ap[1]] if src_ap[1] is not None else [], w=[x_t])
                    P.op("act", lambda e: e.activation(out=junk_bf[:], in_=x_t[:], func=AF.Square, accum_out=st1[:, 0:1]),
                         r=[x_t], w=[junk_bf, st1])
                    P.op("act", lambda e: e.activation(out=st1[:, 1:2], in_=st1[:, 0:1], func=AF.Sqrt, scale=1.0 / D, bias=EPS),
                         r=[st1], w=[st1])
                    P.op("dve", lambda e: e.reciprocal(out=st1[:, 1:2], in_=st1[:, 1:2]), r=[st1], w=[st1])
                    P.op("dve", lambda e: e.scalar_tensor_tensor(out=f32a[:], in0=x_t[:], scalar=st1[:, 1:2], in1=gm_b[:],
                                                                 op0=ALU.mult, op1=ALU.mult), r=[x_t, st1, gm_b], w=[f32a])
                    P.op("dve", lambda e: e.tensor_tensor(out=h_bf[:], in0=f32a[:], in1=shm_b[:], op=ALU.add),
                         r=[f32a, shm_b], w=[h_bf])
                    for k in range(8):
                        P.op("pe", lambda e, k=k: e.transpose(out=ps_bf[:, k * 128:(k + 1) * 128], in_=h_bf[:, k * 128:(k + 1) * 128],
                                                              identity=ident_bf[:]), r=[h_bf, ident_bf], w=[ps_bf])
                    P.op("act", lambda e: e.copy(out=hT[:].rearrange("p k t -> p (k t)"), in_=ps_bf[:]), r=[ps_bf], w=[hT])
                    groups = []
                    for gi in range(3):
                        groups.append(("x", [1536 + (gi * 4 + j) * 128 for j in range(4)], gi * 4))
                    if need_u:
                        groups.append(("u", [g * 128 for g in range(4)], 0))
                    for gidx, (kind, cols, j0) in enumerate(groups):
                        pb = ps[3 + gidx % 2]
                        for jj, c0 in enumerate(cols):
                            for k in range(8):
                                P.op("pe", lambda e, k=k, jj=jj, c0=c0, pb=pb: e.matmul(pb[:, jj * 128:(jj + 1) * 128],
                                                                                       lhsT=w_in[:, k, c0:c0 + 128], rhs=hT[:, k, :],
                                                                                       start=(k == 0), stop=(k == 7)),
                                     r=[hT, w_in], w=[pb])
                        if kind == "u":
                            P.op("act", lambda e, pb=pb: e.copy(out=u_ext[:, :, 15:143], in_=pb[:].rearrange("p (g t) -> p g t", g=4)),
                                 r=[pb], w=[u_ext])
                        else:
                            ce = "dve" if gidx % 2 == 0 else "act"
                            if ce == "act":
                                P.op("act", lambda e, pb=pb, j0=j0: e.copy(out=xbc[:, j0:j0 + 4, 3:131],
                                                                           in_=pb[:].rearrange("p (g t) -> p g t", g=4)), r=[pb], w=[xbc])
                            else:
                                P.op("dve", lambda e, pb=pb, j0=j0: e.tensor_copy(out=xbc[:, j0:j0 + 4, 3:131],
                                                                                  in_=pb[:].rearrange("p (g t) -> p g t", g=4)), r=[pb], w=[xbc])
                    if full:
                        for cb in range(2):
                            for k in range(8):
                                P.op("pe", lambda e, k=k, cb=cb: e.matmul(ps[1 + cb][:], lhsT=hT[:, k, :],
                                                                          rhs=w_in[:, k, 512 + cb * 512:1024 + cb * 512],
                                                                          start=(k == 0), stop=(k == 7)), r=[hT, w_in], w=[ps[1 + cb]])
                    for k in range(8):
                        P.op("pe", lambda e, k=k: e.matmul(ps[0][:, 0:16], lhsT=hT[:, k, :], rhs=w_in[:, k, 3072:3088],
                                                           start=(k == 0), stop=(k == 7)), r=[hT, w_in], w=[ps[0]])
                    if full:
                        for g in range(4):
                            w = 2 << g
                            cur, oth = u_ext, sA
                            step, lo = 1, 1
                            while step < w:
                                dstb = sA if cur is not sA else sB
                                P.op("pool", lambda e, g=g, cur=cur, dstb=dstb, lo=lo, step=step:
                                     e.tensor_tensor(out=dstb[:, g, lo:143], in0=cur[:, g, lo:143], in1=cur[:, g, lo - step:143 - step], op=ALU.add),
                                     r=[cur], w=[dstb])
                                cur = dstb
                                step *= 2
                                lo = 2 * step - 1
                            P.op("pool", lambda e, g=g, cur=cur: e.tensor_tensor(out=ptmp[:, g, :], in0=cur[:, g, 15:143],
                                                                                 in1=invc[:, invc_sel, g, :], op=ALU.mult),
                                 r=[cur, invc], w=[ptmp])
                            P.op("pool", lambda e, g=g: e.tensor_tensor(out=mixed[:, g, :], in0=ptmp[:, g, :], in1=u_ext[:, g, 15:143],
                                                                        op=ALU.subtract), r=[ptmp, u_ext], w=[mixed])
                    if need_u:
                        P.op("pool", lambda e: e.tensor_copy(out=u_ext[:, :, 0:15], in_=u_ext[:, :, 128:143]), r=[u_ext], w=[u_ext])
                    if full:
                        for g in range(4):
                            P.op("pe", lambda e, g=g: e.matmul(ps[5][:, g * 128:(g + 1) * 128], lhsT=pool_w[:, g, :], rhs=mixed[:, g, :],
                                                               start=True, stop=True), r=[pool_w, mixed], w=[ps[5]])
                        for g in range(4):
                            P.op("act", lambda e, g=g: e.activation(out=ypT[:, g, :], in_=ps[5][:, g * 128:(g + 1) * 128], func=AF.Identity,
                                                                    scale=pool_sb[:, g:g + 1], bias=pool_sb[:, 4 + g:5 + g]),
                                 r=[ps[5], pool_sb], w=[ypT])
                    nblk = 12 if full else 10
                    for j in range(nblk):
                        P.op("dve", lambda e, j=j: e.tensor_scalar(out=cacc[:, j, :], in0=xbc[:, j, 0:128], scalar1=conv_wb[:, j, 0:1],
                                                                   scalar2=conv_wb[:, j, 4:5], op0=ALU.mult, op1=ALU.add),
                             r=[xbc, conv_wb], w=[cacc])
                        for k in range(1, 4):
                            P.op("dve", lambda e, j=j, k=k: e.scalar_tensor_tensor(out=cacc[:, j, :], in0=xbc[:, j, k:k + 128],
                                                                                   scalar=conv_wb[:, j, k:k + 1], in1=cacc[:, j, :],
                                                                                   op0=ALU.mult, op1=ALU.add),
                                 r=[xbc, conv_wb, cacc], w=[cacc])
                    P.op("pool", lambda e: e.tensor_copy(out=xbc[:, :, 0:3], in_=xbc[:, :, 128:131]), r=[xbc], w=[xbc])
                    P.op("act", lambda e: e.activation(out=cacc[:, 0:nblk, :], in_=cacc[:, 0:nblk, :], func=AF.Silu), r=[cacc], w=[cacc])
                    if full:
                        P.op("pool", lambda e: e.tensor_copy(out=bct[:], in_=cacc[:, 8:12, :]), r=[cacc], w=[bct])
                    for half in range(2):
                        pb = ps[3 + half]
                        for jj in range(4):
                            j = half * 4 + jj
                            P.op("pe", lambda e, j=j, jj=jj, pb=pb: e.transpose(out=pb[:, jj * 128:(jj + 1) * 128], in_=cacc[:, j, :],
                                                                               identity=ident_f[:]), r=[cacc, ident_f], w=[pb])
                        P.op("act", lambda e, half=half, pb=pb: e.copy(out=xs_tok[:, half * 8:(half + 1) * 8, :].rearrange("p a b -> p (a b)"),
                                                                      in_=pb[:]), r=[pb], w=[xs_tok])
                    for g in range(2):
                        P.op("pe", lambda e, g=g: e.transpose(out=ps[5][:, g * 128:(g + 1) * 128], in_=cacc[:, 8 + g, :], identity=ident_f[:]),
                             r=[cacc, ident_f], w=[ps[5]])
                    P.op("act", lambda e: e.copy(out=B_tok[:], in_=ps[5][:, 0:256]), r=[ps[5]], w=[B_tok])
                    P.op("dve", lambda e: e.tensor_tensor(out=sm[:, 0, :], in0=ps[0][:, 0:16], in1=small[:, 0:16], op=ALU.add),
                         r=[ps[0], small], w=[sm])
                    P.op("act", lambda e: e.activation(out=sm[:, 0, :], in_=sm[:, 0, :], func=AF.Exp), r=[sm], w=[sm])
                    P.op("act", lambda e: e.activation(out=sm[:, 1, :], in_=sm[:, 0, :], func=AF.Ln, bias=1.0), r=[sm], w=[sm])
                    P.op("dve", lambda e: e.tensor_tensor(out=sm[:, 2, :], in0=sm[:, 1, :], in1=small[:, 16:32], op=ALU.mult),
                         r=[sm, small], w=[sm])
                    P.op("pe", lambda e: e.matmul(ps[0][:, 16:32], lhsT=tri[:], rhs=sm[:, 2, :], start=True, stop=True),
                         r=[tri, sm], w=[ps[0]])
                    P.op("pe", lambda e: e.matmul(ps[0][:, 32:48], lhsT=ones_f[:], rhs=sm[:, 2, :], start=True, stop=True),
                         r=[ones_f, sm], w=[ps[0]])
                    P.op("act", lambda e: e.copy(out=sm[:, 3:5, :].rearrange("p a b -> p (a b)"), in_=ps[0][:, 16:48]), r=[ps[0]], w=[sm])
                    P.op("act", lambda e: e.activation(out=sm[:, 5, :], in_=sm[:, 3, :], func=AF.Exp), r=[sm], w=[sm])
                    P.op("dve", lambda e: e.tensor_tensor(out=sm[:, 6, :], in0=sm[:, 4, :], in1=sm[:, 3, :], op=ALU.subtract), r=[sm], w=[sm])
                    P.op("act", lambda e: e.activation(out=sm[:, 6, :], in_=sm[:, 6, :], func=AF.Exp), r=[sm], w=[sm])
                    P.op("act", lambda e: e.activation(out=sm[:, 7, :], in_=sm[:, 4, :], func=AF.Exp), r=[sm], w=[sm])
                    P.op("dve", lambda e: e.tensor_tensor(out=sm2[:], in0=sm[:, 1, :], in1=sm[:, 6, :], op=ALU.mult), r=[sm], w=[sm2])
                    if full:
                        P.op("pool", lambda e: e.tensor_tensor(out=xdt[:], in0=xs_tok[:], in1=bc(sm[:, 1, :].unsqueeze(2), [128, 16, 64]),
                                                               op=ALU.mult), r=[xs_tok, sm], w=[xdt])
                    P.op("pool", lambda e: e.tensor_tensor(out=xdte[:], in0=xs_tok[:], in1=bc(sm2[:].unsqueeze(2), [128, 16, 64]),
                                                           op=ALU.mult), r=[xs_tok, sm2], w=[xdte])
                    if full:
                        for g in range(2):
                            P.op("pe", lambda e, g=g: e.matmul(ps[5][:, 256 + g * 128:384 + g * 128], lhsT=bct[:, g, :], rhs=bct[:, 2 + g, :],
                                                               start=True, stop=True), r=[bct], w=[ps[5]])
                        P.op("dve", lambda e: e.tensor_tensor(out=CBm[:], in0=ps[5][:, 256:512].rearrange("p (g t) -> p g t", g=2),
                                                              in1=bc(tri[:].unsqueeze(1), [128, 2, 128]), op=ALU.mult),
                             r=[ps[5], tri], w=[CBm])
                        t1 = f32a
                        for g in range(2):
                            pb = ps[5 + g]
                            P.op("pe", lambda e, g=g, pb=pb: e.matmul(pb[:], lhsT=bct[:, 2 + g, :], rhs=H_bf[:, g * 512:(g + 1) * 512],
                                                                      start=True, stop=True), r=[bct, H_bf], w=[pb])
                            P.op("dve", lambda e, g=g, pb=pb: e.tensor_tensor(out=t1[:, g * 512:(g + 1) * 512].rearrange("p (a b) -> p a b", a=8),
                                                                              in0=pb[:].rearrange("p (a b) -> p a b", a=8),
                                                                              in1=bc(sm[:, 5, g * 8:(g + 1) * 8].unsqueeze(2), [128, 8, 64]),
                                                                              op=ALU.mult), r=[pb, sm], w=[t1])
                        for q4 in range(4):
                            g = q4 // 2
                            Rb, Db, Mb = Rq[q4 % 2], Dq[q4 % 2], MTq[q4 % 2]
                            pseg = ps[5 + q4 % 2]
                            pyd = ps[3 + g]
                            P.op("dve", lambda e, q4=q4, Rb=Rb: e.tensor_tensor(out=Rb[:], in0=bc(tri[:].unsqueeze(1), [128, 4, 128]),
                                                                               in1=bc(sm[:, 2, 4 * q4:4 * q4 + 4].unsqueeze(2), [128, 4, 128]),
                                                                               op=ALU.mult), r=[tri, sm], w=[Rb])
                            P.op("pe", lambda e, Rb=Rb, pseg=pseg: e.matmul(pseg[:], lhsT=su[:], rhs=Rb[:].rearrange("p a b -> p (a b)"),
                                                                           start=True, stop=True), r=[su, Rb], w=[pseg])
                            P.op("act", lambda e, Db=Db, pseg=pseg: e.activation(out=Db[:].rearrange("p a b -> p (a b)"), in_=pseg[:], func=AF.Exp),
                                 r=[pseg], w=[Db])
                            P.op("pool", lambda e, Db=Db, Mb=Mb, g=g: e.tensor_tensor(out=Mb[:], in0=Db[:], in1=bc(CBm[:, g, :].unsqueeze(1), [128, 4, 128]),
                                                                                     op=ALU.mult), r=[Db, CBm], w=[Mb])
                            for hh in range(4):
                                h = 4 * q4 + hh
                                P.op("pe", lambda e, h=h, hh=hh, Mb=Mb, pyd=pyd: e.matmul(pyd[:, (h % 8) * 64:(h % 8 + 1) * 64], lhsT=Mb[:, hh, :],
                                                                                       rhs=xdt[:, h, :], start=True, stop=True), r=[Mb, xdt], w=[pyd])
                    for g in range(2):
                        pb = ps[5 + g]
                        P.op("pe", lambda e, g=g, pb=pb: e.matmul(pb[:], lhsT=B_tok[:, g * 128:(g + 1) * 128],
                                                                  rhs=xdte[:, g * 8:(g + 1) * 8, :].rearrange("p a b -> p (a b)"),
                                                                  start=True, stop=True), r=[B_tok, xdte], w=[pb])
                    P.op("dve", lambda e: e.tensor_tensor(out=H[:], in0=H[:], in1=bc(sm[:, 7, :].unsqueeze(2), [128, 16, 64]), op=ALU.mult),
                         r=[H, sm], w=[H])
                    for g in range(2):
                        pb = ps[5 + g]
                        P.op("dve", lambda e, g=g, pb=pb: e.tensor_tensor(out=H[:, g * 8:(g + 1) * 8, :], in0=H[:, g * 8:(g + 1) * 8, :],
                                                                          in1=pb[:].rearrange("p (a b) -> p a b", a=8), op=ALU.add),
                             r=[H, pb], w=[H])
                    P.op("pool", lambda e: e.tensor_copy(out=H_bf[:], in_=H[:].rearrange("p a b -> p (a b)")), r=[H], w=[H_bf])
                    if not full:
                        return
                    t1 = f32a
                    for g in range(2):
                        P.op("dve", lambda e, g=g: e.tensor_tensor(out=t1[:, g * 512:(g + 1) * 512], in0=t1[:, g * 512:(g + 1) * 512],
                                                                   in1=ps[3 + g][:], op=ALU.add), r=[t1, ps[3 + g]], w=[t1])
                    P.op("pool", lambda e: e.tensor_tensor(out=f32b[:].rearrange("p (a b) -> p a b", a=16), in0=xs_tok[:],
                                                           in1=bc(small[:, 32:48].unsqueeze(2), [128, 16, 64]), op=ALU.mult),
                         r=[xs_tok, small], w=[f32b])
                    P.op("pool", lambda e: e.tensor_tensor(out=t1[:], in0=t1[:], in1=f32b[:], op=ALU.add), r=[t1, f32b], w=[t1])
                    for g in range(2):
                        P.op("act", lambda e, g=g: e.activation(out=f32b[:, g * 512:(g + 1) * 512], in_=ps[1 + g][:], func=AF.Silu),
                             r=[ps[1 + g]], w=[f32b])
                    P.op("dve", lambda e: e.tensor_tensor(out=t1[:], in0=t1[:], in1=f32b[:], op=ALU.mult), r=[t1, f32b], w=[t1])
                    P.op("act", lambda e: e.activation(out=junk_bf[:], in_=t1[:], func=AF.Square, accum_out=st1[:, 2:3]),
                         r=[t1], w=[junk_bf, st1])
                    P.op("act", lambda e: e.activation(out=st1[:, 3:4], in_=st1[:, 2:3], func=AF.Sqrt, scale=1.0 / D, bias=EPS),
                         r=[st1], w=[st1])
                    P.op("dve", lambda e: e.reciprocal(out=st1[:, 3:4], in_=st1[:, 3:4]), r=[st1], w=[st1])
                    P.op("dve", lambda e: e.scalar_tensor_tensor(out=yn_bf[:], in0=t1[:], scalar=st1[:, 3:4], in1=ssdn_b[:],
                                                                 op0=ALU.mult, op1=ALU.mult), r=[t1, st1, ssdn_b], w=[yn_bf])
                    for k in range(8):
                        P.op("pe", lambda e, k=k: e.transpose(out=ps_bf[:, k * 128:(k + 1) * 128], in_=yn_bf[:, k * 128:(k + 1) * 128],
                                                              identity=ident_bf[:]), r=[yn_bf, ident_bf], w=[ps_bf])
                    P.op("act", lambda e: e.copy(out=ynT[:].rearrange("p k t -> p (k t)"), in_=ps_bf[:]), r=[ps_bf], w=[ynT])
                    for cb in range(2):
                        for kc in range(12):
                            lt = ypT[:, kc, :] if kc < 4 else ynT[:, kc - 4, :]
                            P.op("pe", lambda e, kc=kc, cb=cb, lt=lt: e.matmul(ps[1 + cb][:], lhsT=lt, rhs=w_out[:, kc, cb * 512:(cb + 1) * 512],
                                                                              start=(kc == 0), stop=(kc == 11)),
                                 r=[ypT, ynT, w_out], w=[ps[1 + cb]])
                        P.op("dve", lambda e, cb=cb: e.tensor_tensor(out=x_t[:, cb * 512:(cb + 1) * 512], in0=x_t[:, cb * 512:(cb + 1) * 512],
                                                                     in1=ps[1 + cb][:], op=ALU.add), r=[x_t, ps[1 + cb]], w=[x_t])
                    P.dma("sp", lambda e: e.dma_start(out=dst_ap, in_=x_t[:]), r=[x_t], w=[dst_b])

                maskT = None
                if cfg["own"] == "xs2sel":
                    maskT = P.sb("maskT", [128, 1024], mybir.dt.uint8, sc)
                    P.op("dve", lambda e: e.tensor_copy(out=maskT[:], in_=bc(flag[:, 0:1], [128, 1024])), r=[flag], w=[maskT])
                pre_b = xs2_b if cfg["pre"] == "xs2" else None
                for ti in range(NP):
                    mix_tile(ti, (src_pre[ti * 128:(ti + 1) * 128, :], pre_b), None, None, False, 1 if ti == 0 else 0, False, ti == NP - 1)

                def own_src(ti):
                    rr = slice(ti * 128, (ti + 1) * 128)
                    if cfg["own"] == "x_own":
                        return (x_own[rr, :], None)
                    if cfg["own"] == "xs2":
                        return (xs2[rr, :], xs2_b)
                    assert cfg["own"] == "xs2sel"
                    return (xs2[rr, :], xs2[NO * 128 + ti * 128:NO * 128 + (ti + 1) * 128, :], xs2_b)

                for ti in range(NO):
                    mix_tile(NP + ti, own_src(ti),
                             (x_out if stages == "mix" else xs1)[ti * 128:(ti + 1) * 128, :], xout_b if stages == "mix" else xs1_b, True,
                             (cfg["own0_sel"] if ti == 0 else 0), (ti == 0 and cfg["flag"]), True)
                P.barrier()

            if stages == "mix":
                continue
            with contextlib.ExitStack() as sc:
                NG = 16
                w_q = P.sb("w_q_bf", [128, 8, 2048], BF16, sc)
                keysT = P.sb("keysT_bf", [128, 16, 128], BF16, sc)
                gf_b = P.sb("gf_b", [128, 1024], F32, sc)
                shf_b = P.sb("shf_b", [128, 1024], F32, sc)
                gatef_b = P.sb("gatef_b", [128, 1024], F32, sc)
                nfin_b = P.sb("nfin_b", [128, 1024], F32, sc) if (last and final_norm) else None
                xp = [P.sb("xp%d" % i, [128, 1024], F32, sc) for i in range(2)]
                hf = [P.sb("hf%d" % i, [128, 1024], F32, sc) for i in range(2)]
                f32c = P.sb("f32c", [128, 1024], F32, sc)
                junk2 = P.sb("junk2", [128, 1024], BF16, sc)
                acc = P.sb("acc", [128, 1024], F32, sc)
                junk_bf = P.sb("junk_bf2", [128, 1024], BF16, sc)
                hf_bf = P.sb("hf_bf", [128, 1024], BF16, sc)
                hfT = P.sb("hfT", [128, 8, 128], BF16, sc)
                qT = P.sb("qT", [128, 16, 128], BF16, sc)
                scs = P.sb("scs", [128, 16, 128], F32, sc)
                wk = P.sb("wk", [128, 128], F32, sc)
                wk2 = P.sb("wk2", [128, 256], F32, sc)
                v = P.sb("v", [128, 8, 2, 16], F32, sc)
                iu = P.sb("iu", [128, 8, 2, 16], U32, sc)
                iff = P.sb("iff", [128, 8, 2, 16], F32, sc)
                cand = P.sb("cand", [128, 8, 16, 16], F32, sc)
                cidx = P.sb("cidx", [128, 8, 16, 16], F32, sc)
                tops = P.sb("tops", [128, 8, 16], F32, sc)
                gex = P.sb("gex", [128, 8, 16], F32, sc)
                gz = P.sb("gz", [128, 16], F32, sc)
                gates = [P.sb("gates%d" % i, [128, 128], F32, sc) for i in range(2)]
                idxf = P.sb("idxf", [128, 128], F32, sc)
                idxu = [P.sb("idxu%d" % i, [128, 128], U32, sc) for i in range(2)]
                av = P.sb("av", [128, 128], F32, sc)
                wgt = P.sb("wgt", [128, 128], F32, sc)
                st2 = P.sb("st2", [128, 4], F32, sc)
                ring = [P.sb("ring%d" % i, [128, 1024], BF16, sc) for i in range(NG)]
                diag = [P.sb("diag%d" % i, [128, 128], BF16, sc) for i in range(4)]
                rstate = {"n": 0}

                P.dma("sp", lambda e: e.dma_start(out=gf_b[:], in_=modb[:, 4096:5120]), r=[modb_b], w=[gf_b])
                P.dma("sp", lambda e: e.dma_start(out=f32c[:], in_=W["nffn_b"][:, :]), w=[f32c])
                P.op("dve", lambda e: e.scalar_tensor_tensor(out=gf_b[:], in0=gf_b[:], scalar=1.0, in1=f32c[:],
                                                             op0=ALU.add, op1=ALU.mult), r=[gf_b, f32c], w=[gf_b])
                P.dma("sp", lambda e: e.dma_start(out=shf_b[:], in_=modb[:, 3072:4096]), r=[modb_b], w=[shf_b])
                P.dma("sp", lambda e: e.dma_start(out=gatef_b[:], in_=modb[:, 5120:6144]), r=[modb_b], w=[gatef_b])
                if nfin_b is not None:
                    P.dma("sp", lambda e: e.dma_start(out=nfin_b[:], in_=nfin_d[:, :]), w=[nfin_b])
                stg = [xp[0], xp[1], hf[0], hf[1]]
                wqv = W["w_q"].rearrange("(k p) n -> p k n", p=128)
                i = 0
                for k in range(8):
                    for c0 in range(0, 2048, 1024):
                        sT = stg[i % 4]
                        q = ["sp", "act"][i % 2]
                        P.dma(q, lambda e, sT=sT, k=k, c0=c0: e.dma_start(out=sT[:], in_=wqv[:, k, c0:c0 + 1024]), w=[sT])
                        ce = ["dve", "pool"][i % 2]
                        P.op(ce, lambda e, sT=sT, k=k, c0=c0: e.tensor_copy(out=w_q[:, k, c0:c0 + 1024], in_=sT[:]), r=[sT], w=[w_q])
                        i += 1
                for c0 in range(0, 2048, 1024):
                    sT = stg[i % 4]
                    P.dma("sp", lambda e, sT=sT, c0=c0: e.dma_start(out=sT[:], in_=W["keysT"][:, c0:c0 + 1024]), w=[sT])
                    P.op("dve", lambda e, sT=sT, c0=c0: e.tensor_copy(out=keysT[:].rearrange("p b n -> p (b n)")[:, c0:c0 + 1024], in_=sT[:]),
                         r=[sT], w=[keysT])
                    i += 1

                def peer_A(ti):
                    b = ti % 2
                    x_t, hfx = xp[b], hf[b]
                    P.dma("sp", lambda e: e.dma_start(out=x_t[:], in_=xs1[ti * 128:(ti + 1) * 128, :]), r=[xs1_b], w=[x_t])
                    P.op("act", lambda e: e.activation(out=junk_bf[:], in_=x_t[:], func=AF.Square, accum_out=st2[:, 0:1]),
                         r=[x_t], w=[junk_bf, st2])
                    P.op("act", lambda e: e.activation(out=st2[:, 1:2], in_=st2[:, 0:1], func=AF.Sqrt, scale=1.0 / D, bias=EPS),
                         r=[st2], w=[st2])
                    P.op("dve", lambda e: e.reciprocal(out=st2[:, 1:2], in_=st2[:, 1:2]), r=[st2], w=[st2])
                    P.op("dve", lambda e: e.scalar_tensor_tensor(out=f32c[:], in0=x_t[:], scalar=st2[:, 1:2], in1=gf_b[:],
                                                                 op0=ALU.mult, op1=ALU.mult), r=[x_t, st2, gf_b], w=[f32c])
                    P.op("dve", lambda e: e.tensor_tensor(out=pshf[:], in0=f32c[:], in1=shf_b[:], op=ALU.add), r=[f32c, shf_b], w=[pshf])
                    P.op("act", lambda e: e.copy(out=hf_bf[:], in_=pshf[:]), r=[pshf], w=[hf_bf])
                    for k in range(8):
                        P.op("pe", lambda e, k=k: e.transpose(out=ps_bf[:, k * 128:(k + 1) * 128], in_=hf_bf[:, k * 128:(k + 1) * 128],
                                                              identity=ident_bf[:]), r=[hf_bf, ident_bf], w=[ps_bf])
                    P.op("act", lambda e: e.copy(out=hfT[:].rearrange("p k t -> p (k t)"), in_=ps_bf[:]), r=[ps_bf], w=[hfT])
                    for gq in range(4):
                        pb = ps[gq % 2]
                        for jj in range(4):
                            blk = gq * 4 + jj
                            for k in range(8):
                                P.op("pe", lambda e, k=k, jj=jj, blk=blk, pb=pb: e.matmul(pb[:, jj * 128:(jj + 1) * 128],
                                                                                          lhsT=w_q[:, k, blk * 128:(blk + 1) * 128], rhs=hfT[:, k, :],
                                                                                          start=(k == 0), stop=(k == 7)), r=[w_q, hfT], w=[pb])
                        P.op("act", lambda e, gq=gq, pb=pb: e.copy(out=qT[:, gq * 4:(gq + 1) * 4, :].rearrange("p a b -> p (a b)"), in_=pb[:]),
                             r=[pb], w=[qT])
                    for gq in range(4):
                        pb = ps[gq % 2]
                        for jj in range(4):
                            blk = gq * 4 + jj
                            P.op("pe", lambda e, jj=jj, blk=blk, pb=pb: e.matmul(pb[:, jj * 128:(jj + 1) * 128], lhsT=qT[:, blk, :],
                                                                                 rhs=keysT[:, blk, :], start=True, stop=True),
                                 r=[qT, keysT], w=[pb])
                        P.op("act", lambda e, gq=gq, pb=pb: e.copy(out=scs[:, gq * 4:(gq + 1) * 4, :].rearrange("p a b -> p (a b)"), in_=pb[:]),
                             r=[pb], w=[scs])
                    for blk in range(16):
                        h, s = blk // 2, blk % 2
                        P.op("dve", lambda e, blk=blk, h=h, s=s: e.max(out=v[:, h, s, 0:8], in_=scs[:, blk, :]), r=[scs], w=[v])
                        P.op("dve", lambda e, blk=blk, h=h, s=s: e.match_replace(out=wk[:], in_to_replace=v[:, h, s, 0:8],
                                                                                 in_values=scs[:, blk, :], imm_value=NEG), r=[scs, v], w=[wk])
                        P.op("dve", lambda e, h=h, s=s: e.max(out=v[:, h, s, 8:16], in_=wk[:]), r=[wk], w=[v])
                        P.op("dve", lambda e, blk=blk, h=h, s=s: e.max_index(out=iu[:, h, s, 0:8], in_max=v[:, h, s, 0:8],
                                                                             in_values=scs[:, blk, :]), r=[scs, v], w=[iu])
                        P.op("dve", lambda e, blk=blk, h=h, s=s: e.max_index(out=iu[:, h, s, 8:16], in_max=v[:, h, s, 8:16],
                                                                             in_values=scs[:, blk, :]), r=[scs, v], w=[iu])
                    P.op("dve", lambda e: e.tensor_copy(out=iff[:], in_=iu[:]), r=[iu], w=[iff])
                    P.op("dve", lambda e: e.tensor_scalar(out=iff[:, :, 0, :], in0=iff[:, :, 0, :], scalar1=128.0, scalar2=None, op0=ALU.mult),
                         r=[iff], w=[iff])
                    P.op("dve", lambda e: e.tensor_tensor(out=cand[:], in0=bc(v[:, :, 0, :].unsqueeze(3), [128, 8, 16, 16]),
                                                          in1=bc(v[:, :, 1, :].unsqueeze(2), [128, 8, 16, 16]), op=ALU.add), r=[v], w=[cand])
                    P.op("dve", lambda e: e.tensor_tensor(out=cidx[:], in0=bc(iff[:, :, 0, :].unsqueeze(3), [128, 8, 16, 16]),
                                                           in1=bc(iff[:, :, 1, :].unsqueeze(2), [128, 8, 16, 16]), op=ALU.add), r=[iff], w=[cidx])
                    for h in range(8):
                        ch = cand[:, h, :, :].rearrange("p a b -> p (a b)")
                        P.op("dve", lambda e, h=h, ch=ch: e.max(out=tops[:, h, 0:8], in_=ch), r=[cand], w=[tops])
                        P.op("dve", lambda e, h=h, ch=ch: e.match_replace(out=wk2[:], in_to_replace=tops[:, h, 0:8], in_values=ch, imm_value=NEG),
                             r=[cand, tops], w=[wk2])
                        P.op("dve", lambda e, h=h: e.max(out=tops[:, h, 8:16], in_=wk2[:]), r=[wk2], w=[tops])
                    for h in range(8):
                        ch = cand[:, h, :, :].rearrange("p a b -> p (a b)")
                        ci = cidx[:, h, :, :].rearrange("p a b -> p (a b)")
                        for k in range(16):
                            P.op("dve", lambda e, h=h, k=k, ch=ch, ci=ci: e.scalar_tensor_tensor(out=wk2[:], in0=ch, scalar=tops[:, h, k:k + 1], in1=ci,
                                                                                              op0=ALU.is_equal, op1=ALU.mult,
                                                                                              accum_out=idxf[:, h * 16 + k:h * 16 + k + 1]),
                                 r=[cand, cidx, tops], w=[wk2, idxf])
                    P.op("dve", lambda e: e.tensor_scalar(out=idxf[:], in0=idxf[:], scalar1=16383.0, scalar2=0.0, op0=ALU.min, op1=ALU.max),
                         r=[idxf], w=[idxf])
                    P.op("dve", lambda e: e.tensor_copy(out=idxu[b][:], in_=idxf[:]), r=[idxf], w=[idxu[b]])

                def peer_A2(ti):
                    b = ti % 2
                    P.op("dve", lambda e: e.tensor_tensor(out=gex[:], in0=tops[:], in1=bc(tops[:, :, 0:1], [128, 8, 16]), op=ALU.subtract),
                         r=[tops], w=[gex])
                    P.op("act", lambda e: e.activation(out=gex[:], in_=gex[:], func=AF.Exp), r=[gex], w=[gex])
                    P.op("dve", lambda e: e.tensor_reduce(out=gz[:, 0:8], in_=gex[:], axis=mybir.AxisListType.X, op=ALU.add), r=[gex], w=[gz])
                    P.op("dve", lambda e: e.reciprocal(out=gz[:, 8:16], in_=gz[:, 0:8]), r=[gz], w=[gz])
                    P.op("dve", lambda e: e.tensor_tensor(out=gates[b][:].rearrange("p (h k) -> p h k", h=8), in0=gex[:],
                                                          in1=bc(gz[:, 8:16].unsqueeze(2), [128, 8, 16]), op=ALU.mult), r=[gex, gz], w=[gates[b]])

                def gather(tabk, col, idxT):
                    tab, tabb = TB[(l, tabk)]
                    n = rstate["n"]
                    if n % 4 == 0:
                        P._wait("pool", [], [ring[(n + k) % NG] for k in range(4)])
                    slot = ring[n % NG]
                    rstate["n"] += 1
                    P.dma("pool", lambda e: e.indirect_dma_start(out=slot[:], out_offset=None, in_=tab[:, :],
                                                                 in_offset=bass.IndirectOffsetOnAxis(ap=idxT[:, col:col + 1], axis=0)),
                          r=[idxT, tabb], w=[slot])
                    return slot

                def peer_B(ti):
                    b = ti % 2
                    x_t, hfx = xp[b], hf[b]
                    LOOK = NG - 4
                    order = [("d", j) for j in range(128)] + [("u", j) for j in range(128)]
                    slots = {}
                    issued = 0

                    def issue_upto(n):
                        nonlocal issued
                        while issued < min(n, len(order)):
                            kind, j = order[issued]
                            slots[issued] = gather("e_down" if kind == "d" else "e_up", j, idxu[b])
                            issued += 1

                    for pos, (kind, j) in enumerate(order):
                        if pos == 128 and ti + 1 < NO:
                            peer_A(ti + 1)
                        issue_upto(pos + LOOK)
                        sl = slots.pop(pos)
                        if kind == "d":
                            P.op("dve", lambda e, sl=sl, j=j: e.scalar_tensor_tensor(out=junk2[:], in0=sl[:], scalar=1.0, in1=pshf[:],
                                                                                     op0=ALU.mult, op1=ALU.mult, accum_out=av[:, j:j + 1]),
                                 r=[sl, pshf], w=[junk2, av])
                            if j == 127:
                                P.op("act", lambda e: e.activation(out=wgt[:], in_=av[:], func=AF.Gelu), r=[av], w=[wgt])
                                P.op("dve", lambda e: e.tensor_tensor(out=wgt[:], in0=wgt[:], in1=gates[b][:], op=ALU.mult),
                                     r=[wgt, gates[b]], w=[wgt])
                        else:
                            dg = diag[j % 4]
                            P.op("act", lambda e, dg=dg, j=j: e.activation(out=dg[:], in_=ident_f[:], func=AF.Copy, scale=wgt[:, j:j + 1]),
                                 r=[ident_f, wgt], w=[dg])
                            for hh in range(2):
                                P.op("pe", lambda e, dg=dg, sl=sl, j=j, hh=hh: e.matmul(ps[4 + hh][:], lhsT=dg[:],
                                                                                     rhs=sl[:, hh * 512:(hh + 1) * 512],
                                                                                     start=(j == 0), stop=(j == 127)),
                                     r=[dg, sl], w=[ps[4 + hh]])
                    for hh in range(2):
                        P.op("dve", lambda e, hh=hh: e.tensor_tensor(out=acc[:, hh * 512:(hh + 1) * 512], in0=ps[4 + hh][:],
                                                                     in1=gatef_b[:, hh * 512:(hh + 1) * 512], op=ALU.mult),
                             r=[ps[4 + hh], gatef_b], w=[acc])
                    P.op("dve", lambda e: e.tensor_tensor(out=x_t[:], in0=x_t[:], in1=acc[:], op=ALU.add), r=[x_t, acc], w=[x_t])
                    if last and final_norm:
                        P.op("act", lambda e: e.activation(out=junk_bf[:], in_=x_t[:], func=AF.Square, accum_out=st2[:, 2:3]),
                             r=[x_t], w=[junk_bf, st2])
                        P.op("act", lambda e: e.activation(out=st2[:, 3:4], in_=st2[:, 2:3], func=AF.Sqrt, scale=1.0 / D, bias=EPS),
                             r=[st2], w=[st2])
                        P.op("dve", lambda e: e.reciprocal(out=st2[:, 3:4], in_=st2[:, 3:4]), r=[st2], w=[st2])
                        P.op("dve", lambda e: e.scalar_tensor_tensor(out=x_t[:], in0=x_t[:], scalar=st2[:, 3:4], in1=nfin_b[:],
                                                                     op0=ALU.mult, op1=ALU.mult), r=[x_t, st2, nfin_b], w=[x_t])
                    dstb = xout_b if last else xs2_b
                    P.dma("sp", lambda e: e.dma_start(out=dst_final[ti * 128:(ti + 1) * 128, :], in_=x_t[:]), r=[x_t], w=[dstb])

                peer_A(0)
                peer_A2(0)
                for ti in range(NO):
                    peer_B(ti)
                    if ti + 1 < NO:
                        peer_A2(ti + 1)
                P.barrier()
        P.barrier(["sp"])
    return nc


def _layer_inputs(l, p):
    f = np.float32
    out = {}
    out["w_ada"] = np.ascontiguousarray(p["w_ada"][l], f)
    out["b_ada"] = np.ascontiguousarray(p["b_ada"][l][None, :], f)
    rep = lambda vec: np.ascontiguousarray(np.broadcast_to(np.asarray(vec, f)[None, :], (128, vec.shape[0])))
    out["nmix_b"] = rep(p["norm_mix"][l])
    out["nffn_b"] = rep(p["norm_ffn"][l])
    out["ssdn_b"] = rep(p["ssd_norm"][l])
    out["w_in"] = np.ascontiguousarray(p["w_in"][l], f)
    out["w_out"] = np.ascontiguousarray(p["w_out"][l], f)
    out["w_q"] = np.ascontiguousarray(p["w_query"][l], f)
    out["pool_w"] = np.ascontiguousarray(p["pool_w"][l], f)
    out["pool_sb"] = np.ascontiguousarray(np.concatenate([p["pool_scale"][l].T, p["pool_b"][l].T], axis=1), f)
    cw = p["conv_w"][l].T.reshape(12, 128, 4).transpose(1, 0, 2)
    cb = p["conv_b"][l].reshape(12, 128).T[:, :, None]
    out["conv_wb"] = np.ascontiguousarray(np.concatenate([cw, cb], axis=2).reshape(128, 60), f)
    out["ssd_small"] = rep(np.concatenate([p["dt_bias"][l], p["a_log"][l], p["d_skip"][l]]))
    k = np.stack([p["sub_keys1"][l], p["sub_keys2"][l]], axis=1)
    out["keysT"] = np.ascontiguousarray(k.transpose(3, 0, 1, 2).reshape(128, 2048), f)
    out["e_down"] = np.ascontiguousarray(p["expert_down"][l], f)
    out["e_up"] = np.ascontiguousarray(p["expert_up"][l], f)
    return out


def _invc_tables():
    t = np.arange(128)
    const = np.stack([np.full(128, 1.0 / w) for w in (2, 4, 8, 16)])
    start = np.stack([1.0 / np.minimum(t + 1, w) for w in (2, 4, 8, 16)])
    return const.astype(np.float32), start.astype(np.float32)


def _core_common(c_row, first_half):
    const, start = _invc_tables()
    own0 = start if first_half else const
    invc = np.concatenate([const.reshape(-1), start.reshape(-1), own0.reshape(-1)])
    return {
        "flag": np.full((128, 1), 0.0 if first_half else 1.0, np.float32),
        "invc": np.ascontiguousarray(np.broadcast_to(invc[None, :], (128, 1536)), np.float32),
        "cT": np.ascontiguousarray(c_row.reshape(8, 128).T, np.float32),
    }


_NC_CACHE = {}
FUSED = True


def _get_nc(key, cfgs):
    if key not in _NC_CACHE:
        _NC_CACHE[key] = build_program(cfgs)
    return _NC_CACHE[key]


def kernel(**p):
    p = {k: np.asarray(v) for k, v in p.items()}
    x = p["x"].astype(np.float32)
    Bsz, S, _ = x.shape
    HALF = S // 2
    NT = HALF // 128
    ncores = 2 * Bsz
    nfin = np.ascontiguousarray(np.broadcast_to(p["norm_final"].astype(np.float32)[None, :], (128, D)))
    if FUSED:
        cfgs = [dict(l="0", NP=0, NO=2 * NT, pre=None, own="x_own", flag=False, own0_sel=1, dst="xs2", final_norm=False),
                dict(l="1", NP=NT, NO=NT, pre="xs2", own="xs2sel", flag=True, own0_sel=2, dst="x_out", final_norm=True)]
        nc = _get_nc(("fused", NT), cfgs)
        lw = {}
        for l in range(2):
            lw.update({k + "_%d" % l: v for k, v in _layer_inputs(l, p).items()})
        in_maps = []
        for core in range(ncores):
            b, hh = core // 2, core % 2
            m = dict(lw)
            m.update(_core_common(p["c"][b].astype(np.float32), hh == 0))
            m["x_own"] = np.ascontiguousarray(x[b])
            m["nfin_b"] = nfin
            in_maps.append(m)
        res = run_bass_kernel_spmd(nc, in_maps, core_ids=list(range(ncores)))
        out = np.empty_like(x)
        for core in range(ncores):
            b, hh = core // 2, core % 2
            out[b, hh * HALF:(hh + 1) * HALF] = res.results[core]["x_out"]
        return out
    cur = x
    for l in range(2):
        last = (l == 1)
        cfgs = [dict(l="0", NP=NT, NO=NT, pre="x_pre", own="x_own", flag=True, own0_sel=2, dst="x_out", final_norm=last)]
        nc = _get_nc(("layer", last, NT), cfgs)
        lw = {k + "_0": v for k, v in _layer_inputs(l, p).items()}
        in_maps = []
        for core in range(ncores):
            b, hh = core // 2, core % 2
            m = dict(lw)
            m.update(_core_common(p["c"][b].astype(np.float32), hh == 0))
            m["x_own"] = np.ascontiguousarray(cur[b, hh * HALF:(hh + 1) * HALF])
            m["x_pre"] = np.ascontiguousarray(cur[b, 0:HALF])
            m["nfin_b"] = nfin
            in_maps.append(m)
        res = run_bass_kernel_spmd(nc, in_maps, core_ids=list(range(ncores)))
        nxt = np.empty_like(cur)
        for core in range(ncores):
            b, hh = core // 2, core % 2
            nxt[b, hh * HALF:(hh + 1) * HALF] = res.results[core]["x_out"]
        cur = nxt
    return cur
```
